# Optimizing a Trainium2 kernel written in Bass

```python
import math
import jax, jax.numpy as jnp
from jax import lax
import numpy as np

D_MODEL = 1024
BATCH = 8
SEQ = 4096
DEPTH = 2

D_MIX = D_MODEL
GROUP_DIM = 64
W_A = D_MIX // 4
W_SSD = D_MIX // 2
W_CONF = D_MIX - W_A - W_SSD
A_CONV = 3
SSD_HEAD_DIM = 64
SSD_HEADS = W_SSD // SSD_HEAD_DIM
SSD_GROUPS = 2
SSD_STATE = 128
SSD_CONV = 4
SSD_CHUNK = 128
SSD_XBC = W_SSD + 2 * SSD_GROUPS * SSD_STATE
CONF_KERNEL = 31
IN_COLS = 3 * W_A + W_SSD + SSD_XBC + SSD_HEADS + 2 * W_CONF
D_FF = ((8 * D_MODEL // 3 + 127) // 128) * 128
N_EXPERTS = 8
TOP_K = 2
D_FF_EXPERT = 7 * D_MODEL // 2
MOE_BLOCK = 128
N_DENSE = (DEPTH + 1) // 2
N_MOE = DEPTH // 2
EPS = 1e-5

_SPLITS = list(np.cumsum([W_A, W_A, W_A, W_SSD, SSD_XBC, SSD_HEADS])[:])

kernel_name = "hybrid_shortconv_ssd_conformer_moe"


def rmsnorm(x, w):
    xf = x.astype(jnp.float32)
    y = xf * lax.rsqrt(jnp.mean(xf * xf, axis=-1, keepdims=True) + EPS)
    return (y * w.astype(jnp.float32)).astype(x.dtype)


def layernorm(x, g, b):
    xf = x.astype(jnp.float32)
    mu = jnp.mean(xf, axis=-1, keepdims=True)
    var = jnp.mean(jnp.square(xf - mu), axis=-1, keepdims=True)
    y = (xf - mu) * lax.rsqrt(var + EPS)
    return (y * g.astype(jnp.float32) + b.astype(jnp.float32)).astype(x.dtype)


def causal_dwconv(u, w, b=None):
    k, c = w.shape
    y = lax.conv_general_dilated(
        u, w[:, None, :].astype(u.dtype), window_strides=(1,), padding=[(k - 1, 0)],
        dimension_numbers=('NWC', 'WIO', 'NWC'), feature_group_count=c)
    return y if b is None else y + b


def ssd_chunked(xh, dt, a, bm, cm):
    bsz, seq = xh.shape[:2]
    nc = seq // SSD_CHUNK
    r = SSD_HEADS // SSD_GROUPS
    f32 = jnp.float32
    xc = xh.reshape(bsz, nc, SSD_CHUNK, SSD_GROUPS, r, SSD_HEAD_DIM).astype(f32)
    dtc = dt.reshape(bsz, nc, SSD_CHUNK, SSD_GROUPS, r)
    bc = bm.reshape(bsz, nc, SSD_CHUNK, SSD_GROUPS, SSD_STATE).astype(f32)
    cc = cm.reshape(bsz, nc, SSD_CHUNK, SSD_GROUPS, SSD_STATE).astype(f32)
    xdt = xc * dtc[..., None]
    a_cum = jnp.cumsum(dtc * a.reshape(SSD_GROUPS, r), axis=2)
    causal = jnp.tril(jnp.ones((SSD_CHUNK, SSD_CHUNK), dtype=bool))
    seg = a_cum[:, :, :, None] - a_cum[:, :, None, :]
    decay_ls = jnp.exp(jnp.where(causal[:, :, None, None], seg, -jnp.inf))
    cb = jnp.einsum('bclgn,bcsgn->bclsg', cc, bc)
    y_diag = jnp.einsum('bclsg,bclsgr,bcsgrp->bclgrp', cb, decay_ls, xdt)
    decay_to_end = jnp.exp(a_cum[:, :, -1:] - a_cum)
    states = jnp.einsum('bclgn,bclgr,bclgrp->bcgrpn', bc, decay_to_end, xdt)
    chunk_decay = jnp.exp(a_cum[:, :, -1])

    def step(h, inp):
        dec, st = inp
        return dec[..., None, None] * h + st, h

    h0 = jnp.zeros_like(states[:, 0])
    _, h_prev = lax.scan(step, h0, (jnp.moveaxis(chunk_decay, 1, 0), jnp.moveaxis(states, 1, 0)))
    h_prev = jnp.moveaxis(h_prev, 0, 1)
    y_off = jnp.einsum('bclgn,bcgrpn,bclgr->bclgrp', cc, h_prev, jnp.exp(a_cum))
    return (y_diag + y_off).reshape(bsz, seq, SSD_HEADS, SSD_HEAD_DIM)


def gated_rmsnorm(y, z, w):
    yz = (y * jax.nn.silu(z)).astype(jnp.float32)
    shp = yz.shape
    yg = yz.reshape(shp[:-1] + (SSD_GROUPS, shp[-1] // SSD_GROUPS))
    yg = yg * lax.rsqrt(jnp.mean(yg * yg, axis=-1, keepdims=True) + EPS)
    return (yg.reshape(shp) * w.astype(jnp.float32)).astype(y.dtype)


def hybrid_mixer(h, w_in, conv_a_w, conv_ssd_w, conv_ssd_b, dt_bias, a_log, d_skip,
                 ssd_norm_w, conv_conf_w, conv_conf_b, conf_ln_g, conf_ln_b, w_out):
    bsz, seq, _ = h.shape
    proj = h @ w_in
    a_b, a_c, a_x, s_z, s_xbc, s_dt, c_glu = jnp.split(proj, _SPLITS, axis=-1)
    y_a = a_b * causal_dwconv(a_c * a_x, conv_a_w)
    xbc = jax.nn.silu(causal_dwconv(s_xbc, conv_ssd_w, conv_ssd_b))
    s_x, s_b, s_c = jnp.split(xbc, [W_SSD, W_SSD + SSD_GROUPS * SSD_STATE], axis=-1)
    dt = jax.nn.softplus(s_dt.astype(jnp.float32) + dt_bias.astype(jnp.float32))
    a = -jnp.exp(a_log.astype(jnp.float32))
    xh = s_x.reshape(bsz, seq, SSD_HEADS, SSD_HEAD_DIM)
    y_s = ssd_chunked(xh, dt, a,
                      s_b.reshape(bsz, seq, SSD_GROUPS, SSD_STATE),
                      s_c.reshape(bsz, seq, SSD_GROUPS, SSD_STATE)).astype(h.dtype)
    y_s = (y_s + d_skip[:, None] * xh).reshape(bsz, seq, W_SSD)
    y_s = gated_rmsnorm(y_s, s_z, ssd_norm_w)
    g = c_glu[..., :W_CONF] * jax.nn.sigmoid(c_glu[..., W_CONF:])
    g = causal_dwconv(g, conv_conf_w, conv_conf_b)
    y_c = jax.nn.silu(layernorm(g, conf_ln_g, conf_ln_b))
    return jnp.concatenate([y_a, y_s, y_c], axis=-1) @ w_out


def swiglu(h, w_gate, w_up, w_down):
    return (jax.nn.silu(h @ w_gate) * (h @ w_up)) @ w_down


def moe_swiglu(h, w_router, w_gate, w_up, w_down):
    bsz, seq, d = h.shape
    t = bsz * seq
    m = t * TOP_K
    hf = h.reshape(t, d)
    logits = (hf @ w_router).astype(jnp.float32)
    top_vals, top_idx = lax.top_k(logits, TOP_K)
    gates = jax.nn.softmax(top_vals, axis=-1)
    flat_e = top_idx.reshape(m)
    order = jnp.argsort(flat_e)
    sorted_e = flat_e[order]
    tok = order // TOP_K
    sizes = jnp.bincount(flat_e, length=N_EXPERTS).astype(jnp.int32)
    padded = ((sizes + MOE_BLOCK - 1) // MOE_BLOCK) * MOE_BLOCK
    ends = jnp.cumsum(padded)
    starts_pad = ends - padded
    starts_sorted = jnp.cumsum(sizes) - sizes
    dest = starts_pad[sorted_e] + (jnp.arange(m, dtype=jnp.int32) - starts_sorted[sorted_e])
    cap = m + N_EXPERTS * MOE_BLOCK
    n_blocks = cap // MOE_BLOCK
    buf = jnp.zeros((cap, d), hf.dtype).at[dest].set(hf[tok])
    blk_e = jnp.minimum(jnp.searchsorted(ends, jnp.arange(n_blocks, dtype=jnp.int32) * MOE_BLOCK,
                                         side='right'), N_EXPERTS - 1)

    def expert_block(args):
        xb, e = args
        return (jax.nn.silu(xb @ w_gate[e]) * (xb @ w_up[e])) @ w_down[e]

    ybuf = lax.map(expert_block, (buf.reshape(n_blocks, MOE_BLOCK, d), blk_e)).reshape(cap, d)
    y = ybuf[dest] * gates.reshape(m)[order][:, None].astype(ybuf.dtype)
    out = jnp.zeros_like(hf).at[tok].add(y)
    return out.reshape(bsz, seq, d)


def setup_inputs(seed: int = 0) -> dict:
    key = jax.random.key(seed)
    ks = jax.random.split(key, 32)
    f32 = jnp.float32

    def nrm(k, shape, scale):
        return jax.random.normal(k, shape, f32) * scale

    def gain(k, shape):
        return 1.0 + 0.02 * jax.random.normal(k, shape, f32)

    dt0 = jnp.exp(jax.random.uniform(ks[5], (DEPTH, SSD_HEADS), f32,
                                     math.log(1e-3), math.log(1e-1)))
    dt_bias = dt0 + jnp.log(-jnp.expm1(-dt0))
    a_log = jnp.log(jax.random.uniform(ks[6], (DEPTH, SSD_HEADS), f32, 1.0, 16.0))
    return {
        'x': jax.random.normal(ks[0], (BATCH, SEQ, D_MODEL), f32),
        'norm_mix': gain(ks[1], (DEPTH, D_MODEL)),
        'w_in': nrm(ks[2], (DEPTH, D_MODEL, IN_COLS), D_MODEL ** -0.5),
        'conv_a_w': nrm(ks[3], (DEPTH, A_CONV, W_A), A_CONV ** -0.5),
        'conv_ssd_w': nrm(ks[4], (DEPTH, SSD_CONV, SSD_XBC), SSD_CONV ** -0.5),
        'conv_ssd_b': nrm(ks[7], (DEPTH, SSD_XBC), 0.02),
        'dt_bias': dt_bias,
        'a_log': a_log,
        'd_skip': 1.0 + 0.1 * jax.random.normal(ks[8], (DEPTH, SSD_HEADS), f32),
        'ssd_norm_w': gain(ks[9], (DEPTH, W_SSD)),
        'conv_conf_w': nrm(ks[10], (DEPTH, CONF_KERNEL, W_CONF), CONF_KERNEL ** -0.5),
        'conv_conf_b': nrm(ks[11], (DEPTH, W_CONF), 0.02),
        'conf_ln_g': gain(ks[12], (DEPTH, W_CONF)),
        'conf_ln_b': nrm(ks[13], (DEPTH, W_CONF), 0.02),
        'w_out': nrm(ks[14], (DEPTH, D_MIX, D_MODEL), D_MIX ** -0.5),
        'norm_ffn': gain(ks[15], (DEPTH, D_MODEL)),
        'ffn_w_gate': nrm(ks[16], (N_DENSE, D_MODEL, D_FF), D_MODEL ** -0.5),
        'ffn_w_up': nrm(ks[17], (N_DENSE, D_MODEL, D_FF), D_MODEL ** -0.5),
        'ffn_w_down': nrm(ks[18], (N_DENSE, D_FF, D_MODEL), D_FF ** -0.5),
        'moe_router': nrm(ks[19], (N_MOE, D_MODEL, N_EXPERTS), D_MODEL ** -0.5),
        'moe_w_gate': nrm(ks[20], (N_MOE, N_EXPERTS, D_MODEL, D_FF_EXPERT), D_MODEL ** -0.5),
        'moe_w_up': nrm(ks[21], (N_MOE, N_EXPERTS, D_MODEL, D_FF_EXPERT), D_MODEL ** -0.5),
        'moe_w_down': nrm(ks[22], (N_MOE, N_EXPERTS, D_FF_EXPERT, D_MODEL), D_FF_EXPERT ** -0.5),
        'norm_final': gain(ks[23], (D_MODEL,)),
    }


def reference(x, norm_mix, w_in, conv_a_w, conv_ssd_w, conv_ssd_b, dt_bias, a_log, d_skip,
              ssd_norm_w, conv_conf_w, conv_conf_b, conf_ln_g, conf_ln_b, w_out, norm_ffn,
              ffn_w_gate, ffn_w_up, ffn_w_down, moe_router, moe_w_gate, moe_w_up, moe_w_down,
              norm_final):
    for i in range(DEPTH):
        h = rmsnorm(x, norm_mix[i])
        x = x + hybrid_mixer(h, w_in[i], conv_a_w[i], conv_ssd_w[i], conv_ssd_b[i], dt_bias[i],
                             a_log[i], d_skip[i], ssd_norm_w[i], conv_conf_w[i], conv_conf_b[i],
                             conf_ln_g[i], conf_ln_b[i], w_out[i])
        h = rmsnorm(x, norm_ffn[i])
        j = i // 2
        if i % 2 == 0:
            x = x + swiglu(h, ffn_w_gate[j], ffn_w_up[j], ffn_w_down[j])
        else:
            x = x + moe_swiglu(h, moe_router[j], moe_w_gate[j], moe_w_up[j], moe_w_down[j])
    return rmsnorm(x, norm_final)
```

```python
import contextlib
import numpy as np
import concourse.bass as bass
import concourse.mybir as mybir
from concourse.bass_utils import run_bass_kernel_spmd

F32 = mybir.dt.float32
BF16 = mybir.dt.bfloat16
I32 = mybir.dt.int32
U32 = mybir.dt.uint32
AF = mybir.ActivationFunctionType
ALU = mybir.AluOpType
AX = mybir.AxisListType
PE_ENG = mybir.EngineType.PE

L = 4096
D = 1024
NT = L // 128
EPS = 1e-5
IN_COLS = 2824
DFF = 2816
NFF = DFF // 128
NE = 8
DFE = 3584
NFE = DFE // 128


class T:
    __slots__ = ("name", "w", "r", "excl")

    def __init__(self, name="", excl=False):
        self.name = name
        self.w = None
        self.r = {}
        self.excl = excl


def TL(n, name=""):
    return [T(f"{name}{i}") for i in range(n)]


class AS:
    def __init__(self, nc, es):
        self.nc = nc
        self.eng = {"pe": nc.tensor, "act": nc.scalar, "dve": nc.vector, "pool": nc.gpsimd, "sp": nc.sync}
        self.sem = {k: es.enter_context(nc.semaphore("s_" + k)) for k in ("pe", "act", "dve", "pool")}
        self.cnt = {k: 0 for k in self.sem}
        self.dq = {}
        for q, n in (("sp", 16), ("pool", 12), ("act", 6)):
            self.dq[q] = {"sems": [es.enter_context(nc.semaphore(f"d_{q}{i}")) for i in range(n)],
                          "cnt": [0] * n, "next": 0}
        self.known = {k: {} for k in self.eng}
        self.snap = {}
        self.nwait = 0
        self.nop = 0
        self.halt = False

    def _semof(self, key):
        if isinstance(key, tuple):
            return self.dq[key[0]]["sems"][key[1]]
        return self.sem[key]

    def _need(self, eng, deps):
        if self.halt:
            return
        kn = self.known[eng]
        for key, val in deps.items():
            if kn.get(key, 0) >= val:
                continue
            if key == eng and eng == "pe":
                continue
            self.eng[eng].wait_ge(self._semof(key), val)
            self.nwait += 1
            kn[key] = val
            sn = self.snap.get((key, val))
            if sn:
                for k2, v2 in sn.items():
                    if kn.get(k2, 0) < v2:
                        kn[k2] = v2

    @staticmethod
    def _deps(reads, writes, eng=None):
        deps = {}

        def add(kv):
            k, v = kv
            if deps.get(k, 0) < v:
                deps[k] = v
        for t in reads:
            if t.w:
                add(t.w)
            if t.excl:
                for kv in t.r.items():
                    if kv[0] != eng:
                        add(kv)
        for t in writes:
            if t.w:
                add(t.w)
            for kv in t.r.items():
                add(kv)
        return deps

    def op(self, eng, fn, r=(), w=()):
        if self.halt:
            return None
        self._need(eng, self._deps(r, w, eng))
        inst = fn(self.eng[eng])
        self.cnt[eng] += 1
        c = self.cnt[eng]
        inst.then_inc(self.sem[eng], 1)
        self.nop += 1
        self.snap[(eng, c)] = dict(self.known[eng])
        for t in r:
            t.r[eng] = c
        for t in w:
            t.w = (eng, c)
            t.r = {}
        return inst

    def dma(self, q, out, in_, r=(), w=(), indirect=None):
        if self.halt:
            return None
        dq = self.dq[q]
        i = dq["next"]
        dq["next"] = (i + 1) % len(dq["sems"])
        key = (q, i)
        deps = self._deps(r, w)
        if dq["cnt"][i] > 0:
            deps[key] = max(deps.get(key, 0), dq["cnt"][i])
        self._need(q, deps)
        e = self.eng[q]
        if indirect is None:
            inst = e.dma_start(out=out, in_=in_)
        else:
            inst = e.indirect_dma_start(out=out, in_=in_, **indirect)
        dq["cnt"][i] += 16
        v = dq["cnt"][i]
        inst.then_inc(dq["sems"][i], 16)
        self.nop += 1
        self.snap[(key, v)] = dict(self.known[q])
        for t in r:
            t.r[key] = v
        for t in w:
            t.w = (key, v)
            t.r = {}
        return inst

    def finish(self):
        self.halt = False
        deps = {}
        for q, dq in self.dq.items():
            for i, c in enumerate(dq["cnt"]):
                if c > 0:
                    deps[(q, i)] = c
        self._need("sp", deps)
        self._need("sp", {k: c for k, c in self.cnt.items() if c > 0})


_UID = [0]


class Ring:
    def __init__(self, nc, es, name, shape, dtype, n):
        _UID[0] += 1
        self.bufs = [es.enter_context(nc.sbuf_tensor(f"r{_UID[0]}_{name}{i}", shape, dtype)) for i in range(n)]
        self.ts = TL(n, name)
        self.i = 0

    def next(self):
        i = self.i
        self.i = (i + 1) % len(self.bufs)
        return self.bufs[i], self.ts[i]


def bc(ap, shape):
    return ap.to_broadcast(shape)


CAP = 12
CAPROWS = CAP * 128
NBLK = NE * CAP
NROW = NBLK * 128


class _Stop(Exception):
    pass


def build(stop_after=99, debug=False):
    nc = bass.Bass("TRN2", target_bir_lowering=False)

    def din(name, shape, dt=F32):
        return nc.dram_tensor(name, shape, dt, kind="ExternalInput").ap()

    def dscr(name, shape, dt=F32):
        kind = "ExternalOutput" if debug else "Internal"
        return nc.dram_tensor(name, shape, dt, kind=kind).ap()

    x_in = din("x", [L, D])
    cst_d = din("cst", [128, 768])
    nw_d = din("nw", [128, 5, D])
    win_d = [din("win0", [D, IN_COLS]), din("win1", [D, IN_COLS])]
    cwa_d = din("cwa", [128, 2, 2, 3])
    cws_d = din("cws", [128, 2, 8, 5])
    cwc_d = din("cwc", [128, 2, 2, 34])
    hv_d = din("hv", [128, 2, 3, 8])
    snw_d = din("snw", [128, 2, 512])
    wout_d = din("wout", [2, D, D])
    ffg_d = din("ffg", [D, DFF])
    ffu_d = din("ffu", [D, DFF])
    ffd_d = din("ffd", [DFF, D])
    wr_d = din("wr", [D, NE])
    need_moe = stop_after > 12
    if need_moe:
        mg_d = din("mg", [NE, D, DFE])
        mu_d = din("mu", [NE, D, DFE])
        md_d = din("md", [NE, DFE, D])
    else:
        mg_d = nc.dram_tensor("mg", [NE, D, DFE], F32, kind="Internal").ap()
        mu_d = nc.dram_tensor("mu", [NE, D, DFE], F32, kind="Internal").ap()
        md_d = nc.dram_tensor("md", [NE, DFE, D], F32, kind="Internal").ap()
    out_d = nc.dram_tensor("out", [L, D], F32, kind="ExternalOutput").ap()

    xres_d = dscr("xres", [L, D])
    yT_d = dscr("yT", [D, L], BF16)
    hm_d = nc.dram_tensor("hm", [NROW + 128, D], BF16, kind="Internal").ap()
    yb_d = nc.dram_tensor("yb", [NROW + 128, D], F32, kind="Internal").ap()

    xres_T = TL(NT, "xres")
    yT_T = TL(8, "yT")

    with contextlib.ExitStack() as es:
        S = AS(nc, es)
        op, dma = S.op, S.dma

        def sb(name, shape, dt=F32, stack=es):
            _UID[0] += 1
            return stack.enter_context(nc.sbuf_tensor(f"t{_UID[0]}_{name}", shape, dt))

        cst = sb("cst", [128, 768]); cst_T = T("cst")
        identb = sb("identb", [128, 128], BF16); identb_T = T("identb")
        nwb = sb("nwb", [128, D]); nwb_T = T("nwb")
        epsc = sb("epsc", [128, 1]); epsc_T = T("epsc")
        PSF = [es.enter_context(nc.psum_tensor(f"psf{i}", [128, 512], F32)) for i in range(6)]
        PSB = [es.enter_context(nc.psum_tensor(f"psb{i}", [128, 1024], BF16)) for i in range(2)]
        PSF_T = [T(f"psf{i}", excl=True) for i in range(6)]
        PSB_T = [T(f"psb{i}", excl=True) for i in range(2)]
        GT = sb("m_GT", [128, NT, 2], F32)
        desti = sb("m_desti", [128, 2, NT], U32)
        rt_T = T("route")
        hstack = contextlib.ExitStack()
        HT = sb("HT", [128, 8, L], BF16, hstack); HT_T = TL(NT, "HT")

        ident = cst[:, 0:128]
        LEm = cst[:, 128:256]
        LTs = cst[:, 256:384]
        SUm = cst[:, 384:512]
        ones = cst[:, 512:640]
        ones256 = cst[:, 640:768]

        op("dve", lambda e: e.memset(epsc[:], EPS), w=[epsc_T])
        dma("sp", cst[:], cst_d[:], w=[cst_T])
        op("dve", lambda e: e.tensor_copy(out=identb[:], in_=ident), r=[cst_T], w=[identb_T])

        def acopy(out, in_, r, w, eng="act"):
            if eng == "act":
                op("act", lambda e: e.activation(out=out, in_=in_, func=AF.Copy), r=r, w=w)
            else:
                op(eng, lambda e: e.tensor_copy(out=out, in_=in_), r=r, w=w)

        def barrier():
            allc = {k: c for k, c in S.cnt.items() if c > 0}
            for q, dq in S.dq.items():
                for i, c in enumerate(dq["cnt"]):
                    if c > 0:
                        allc[(q, i)] = c
            for e_ in ("pe", "act", "dve", "pool", "sp"):
                S._need(e_, allc)

        def rstd_from_ss(ss, n, tmp, rs, t_):
            op("act", lambda e: e.activation(out=tmp, in_=ss, func=AF.Ln, scale=1.0 / n, bias=epsc[:, 0:1]),
               r=[t_, epsc_T], w=[t_])
            op("act", lambda e: e.activation(out=rs, in_=tmp, func=AF.Exp, scale=-0.5), r=[t_], w=[t_])

        def norm_tile(xt, xt_T, out, out_T, smr, junk, junk_T):
            sm, sm_T = smr.next()
            op("act", lambda e: e.activation(out=junk[:], in_=xt, func=AF.Square, accum_out=sm[:, 0:1]),
               r=[xt_T], w=[junk_T, sm_T])
            rstd_from_ss(sm[:, 0:1], D, sm[:, 1:2], sm[:, 2:3], sm_T)
            op("dve", lambda e: e.scalar_tensor_tensor(out=out, in0=xt, scalar=sm[:, 2:3], in1=nwb[:],
                                                       op0=ALU.mult, op1=ALU.mult),
               r=[xt_T, sm_T, nwb_T], w=[out_T])

        def to_HT(hn, hn_T, i):
            pb, pb_T = PSB[i % 2], PSB_T[i % 2]
            for k in range(8):
                op("pe", lambda e, k=k: e.transpose(out=pb[:, k * 128:(k + 1) * 128], in_=hn[:, k * 128:(k + 1) * 128],
                                                    identity=identb[:]),
                   r=[hn_T, identb_T], w=[pb_T])
            acopy(HT[:, :, i * 128:(i + 1) * 128], pb[:].rearrange("p (k t) -> p k t", k=8), [pb_T], [HT_T[i]])

        def stop_at(x):
            if stop_after <= x:
                S.halt = True

        try:
            for layer in range(2):
                res_src = x_in if layer == 0 else xres_d
                if layer == 0:
                    with contextlib.ExitStack() as ps:
                        xin = Ring(nc, ps, "a_x", [128, D], F32, 3)
                        hnr = Ring(nc, ps, "a_hn", [128, D], BF16, 2)
                        smr = Ring(nc, ps, "a_sm", [128, 4], F32, 3)
                        junk = sb("a_junk", [128, D], BF16, ps); junk_T = T()
                        dma("sp", nwb[:], nw_d[:, 0, :], w=[nwb_T])
                        for i in range(NT):
                            xt, xt_T = xin.next()
                            dma("sp", xt[:], x_in[i * 128:(i + 1) * 128, :], w=[xt_T])
                            hn, hn_T = hnr.next()
                            norm_tile(xt[:], xt_T, hn[:], hn_T, smr, junk, junk_T)
                            to_HT(hn, hn_T, i)
                        barrier()
                stop_at(0)

                with contextlib.ExitStack() as ps:
                    FB = [sb(f"b_F{i}", [128, L + 32], F32, ps) for i in range(3)]
                    FB_T = TL(3, "F")
                    HB = [sb(f"b_H{i}", [128, L], BF16, ps) for i in range(4)]
                    HB_T = TL(4, "H")
                    wch = Ring(nc, ps, "b_w", [128, 8, 512], BF16, 2)
                    cwa = sb("b_cwa", [128, 2, 3], F32, ps); cwa_T = T()
                    cws = sb("b_cws", [128, 8, 5], F32, ps); cws_T = T()
                    cwc = sb("b_cwc", [128, 2, 34], F32, ps); cwc_T = T()
                    hv = sb("b_hv", [128, 3, 8], F32, ps); hv_T = T()
                    snw = sb("b_snw", [128, 512], F32, ps); snw_T = T()
                    acs = contextlib.ExitStack()
                    ps.callback(acs.close)
                    tmpf = Ring(nc, acs, "b_tmpf", [128, 512], F32, 4)
                    dma("sp", cwa[:], cwa_d[:, layer], w=[cwa_T])
                    dma("sp", cws[:], cws_d[:, layer], w=[cws_T])
                    dma("sp", cwc[:], cwc_d[:, layer], w=[cwc_T])
                    dma("sp", hv[:], hv_d[:, layer], w=[hv_T])
                    dma("sp", snw[:], snw_d[:, layer], w=[snw_T])
                    for f in range(3):
                        op("pool", lambda e, f=f: e.memset(FB[f][:, 0:32], 0.0), w=[FB_T[f]])
                    win = win_d[layer].rearrange("(kc p) c -> p kc c", p=128)
                    psrot = [0]

                    def load_w(col0, nch):
                        wt, wt_T = wch.next()
                        dma("pool", wt[:, :, 0:nch * 128], win[:, :, col0:col0 + nch * 128], w=[wt_T])
                        return [(wt[:, :, k * 128:(k + 1) * 128], wt_T) for k in range(nch)]

                    def proj_blk(wt, wt_T, blk):
                        bi = psrot[0]
                        psrot[0] = (bi + 1) % 4
                        p_, p_T = PSF[bi], PSF_T[bi]
                        rT = [wt_T] + HT_T[blk * 4:(blk + 1) * 4]
                        for kc in range(8):
                            op("pe", lambda e, kc=kc: e.matmul(p_[:], lhsT=wt[:, kc, :], rhs=HT[:, kc, blk * 512:(blk + 1) * 512],
                                                               start=(kc == 0), stop=(kc == 7)), r=rT, w=[p_T])
                        return p_, p_T

                    def conv_taps(src, src_T, K, wts, wT, acc, acc_T, bias=None, step=2048):
                        for s0 in range(0, L, step):
                            n = step
                            for j in range(K):
                                o = 32 - (K - 1) + j + s0
                                if j == 0:
                                    if bias is None:
                                        op("dve", lambda e, o=o, s0=s0: e.tensor_scalar(
                                            out=acc[:, s0:s0 + n], in0=src[:, o:o + n], scalar1=wts[:, 0:1], scalar2=None,
                                            op0=ALU.mult), r=[src_T, wT], w=[acc_T])
                                    else:
                                        op("dve", lambda e, o=o, s0=s0: e.tensor_scalar(
                                            out=acc[:, s0:s0 + n], in0=src[:, o:o + n], scalar1=wts[:, 0:1], scalar2=bias,
                                            op0=ALU.mult, op1=ALU.add), r=[src_T, wT], w=[acc_T])
                                else:
                                    op("dve", lambda e, o=o, s0=s0, j=j: e.scalar_tensor_tensor(
                                        out=acc[:, s0:s0 + n], in0=src[:, o:o + n], scalar=wts[:, j:j + 1], in1=acc[:, s0:s0 + n],
                                        op0=ALU.mult, op1=ALU.add), r=[src_T, wT, acc_T], w=[acc_T])

                    for j in range(2):
                        (wb_, wb_T), (wc_, wc_T), (wx_, wx_T) = load_w(j * 384, 3)
                        for blk in range(8):
                            sl = slice(blk * 512, (blk + 1) * 512)
                            slp = slice(32 + blk * 512, 32 + (blk + 1) * 512)
                            pc, pc_T = proj_blk(wc_, wc_T, blk)
                            tf, tf_T = tmpf.next()
                            acopy(tf[:], pc[:], [pc_T], [tf_T])
                            px, px_T = proj_blk(wx_, wx_T, blk)
                            op("dve", lambda e: e.tensor_tensor(out=FB[0][:, slp], in0=tf[:], in1=px[:], op=ALU.mult),
                               r=[tf_T, px_T], w=[FB_T[0]])
                            pb_, pb_T = proj_blk(wb_, wb_T, blk)
                            acopy(FB[1][:, sl], pb_[:], [pb_T], [FB_T[1]])
                        conv_taps(FB[0], FB_T[0], 3, cwa[:, j, :], cwa_T, FB[2], FB_T[2])
                        for s0 in range(0, L, 2048):
                            op("dve", lambda e, s0=s0: e.tensor_tensor(out=HB[j][:, s0:s0 + 2048], in0=FB[2][:, s0:s0 + 2048],
                                                                       in1=FB[1][:, s0:s0 + 2048], op=ALU.mult),
                               r=[FB_T[2], FB_T[1]], w=[HB_T[j]])
                        dma("sp", yT_d[j * 128:(j + 1) * 128, :], HB[j][:], r=[HB_T[j]], w=[yT_T[j]])

                    if layer == 0:
                        stop_at(0.2)
                    for j in range(2):
                        (wa_, wa_T), (wg_, wg_T) = load_w(768 + j * 256, 2)
                        for blk in range(8):
                            slp = slice(32 + blk * 512, 32 + (blk + 1) * 512)
                            pg, pg_T = proj_blk(wg_, wg_T, blk)
                            tf, tf_T = tmpf.next()
                            op("act", lambda e: e.activation(out=tf[:], in_=pg[:], func=AF.Sigmoid), r=[pg_T], w=[tf_T])
                            pa, pa_T = proj_blk(wa_, wa_T, blk)
                            op("dve", lambda e: e.tensor_tensor(out=FB[0][:, slp], in0=tf[:], in1=pa[:], op=ALU.mult),
                               r=[tf_T, pa_T], w=[FB_T[0]])
                        conv_taps(FB[0], FB_T[0], 31, cwc[:, j, 0:31], cwc_T, FB[1 + j], FB_T[1 + j], bias=cwc[:, j, 31:32])
                    for blk in range(8):
                        sl = slice(blk * 512, (blk + 1) * 512)
                        sq = []
                        for j in range(2):
                            tf, tf_T = tmpf.next()
                            op("act", lambda e, j=j: e.activation(out=tf[:], in_=FB[1 + j][:, sl], func=AF.Square),
                               r=[FB_T[1 + j]], w=[tf_T])
                            sq.append((tf, tf_T))
                        pm, pm_T = PSF[4], PSF_T[4]
                        pe2, pe2_T = PSF[5], PSF_T[5]
                        for j in range(2):
                            op("pe", lambda e, j=j: e.matmul(pm[:], lhsT=ones256, rhs=FB[1 + j][:, sl], start=(j == 0), stop=(j == 1)),
                               r=[cst_T, FB_T[1 + j]], w=[pm_T])
                        for j in range(2):
                            op("pe", lambda e, j=j: e.matmul(pe2[:], lhsT=ones256, rhs=sq[j][0][:], start=(j == 0), stop=(j == 1)),
                               r=[cst_T, sq[j][1]], w=[pe2_T])
                        mean, mean_T = tmpf.next()
                        acopy(mean[:], pm[:], [pm_T], [mean_T])
                        var, var_T = sq[0]
                        op("dve", lambda e: e.tensor_tensor(out=var[:], in0=mean[:], in1=mean[:], op=ALU.mult), r=[mean_T], w=[var_T])
                        op("dve", lambda e: e.tensor_tensor(out=var[:], in0=pe2[:], in1=var[:], op=ALU.subtract),
                           r=[pe2_T, var_T], w=[var_T])
                        op("dve", lambda e: e.tensor_scalar(out=var[:], in0=var[:], scalar1=EPS, scalar2=None, op0=ALU.add),
                           r=[var_T], w=[var_T])
                        op("act", lambda e: e.activation(out=var[:], in_=var[:], func=AF.Sqrt), r=[var_T], w=[var_T])
                        op("dve", lambda e: e.reciprocal(out=var[:], in_=var[:]), r=[var_T], w=[var_T])
                        t2, t2_T = sq[1]
                        for j in range(2):
                            op("dve", lambda e, j=j: e.tensor_tensor(out=t2[:], in0=FB[1 + j][:, sl], in1=mean[:], op=ALU.subtract),
                               r=[FB_T[1 + j], mean_T], w=[t2_T])
                            op("dve", lambda e: e.tensor_tensor(out=t2[:], in0=t2[:], in1=var[:], op=ALU.mult),
                               r=[t2_T, var_T], w=[t2_T])
                            op("act", lambda e, j=j: e.activation(out=HB[2 + j][:, sl], in_=t2[:], func=AF.Silu,
                                                                  scale=cwc[:, j, 32:33], bias=cwc[:, j, 33:34]),
                               r=[t2_T, cwc_T], w=[HB_T[2 + j]])
                    for j in range(2):
                        dma("sp", yT_d[768 + j * 128:768 + (j + 1) * 128, :], HB[2 + j][:], r=[HB_T[2 + j]], w=[yT_T[6 + j]])

                    if layer == 0:
                        stop_at(0.4)
                    barrier()
                    acs.close()
                    with contextlib.ExitStack() as ss_:
                        rYst = Ring(nc, ss_, "s_yst", [128, 2, 256], BF16, 2)
                        wzz = sb("s_wz", [128, 8, 520], BF16, ss_); wz_T = T()
                        dma("pool", wzz[:], win[:, :, 2304:2824], w=[wz_T])
                        wdt_T = wz_T
                        dtt = sb("s_dt", [128, NT, 8], F32, ss_); dtt_T = T()
                        dtA = sb("s_dtA", [128, NT, 8], F32, ss_); dtA_T = T()
                        acum = sb("s_acum", [128, NT, 8], F32, ss_); acum_T = T()
                        Eall = sb("s_E", [128, NT, 8], F32, ss_); Eall_T = T()
                        cdall = sb("s_cd", [128, NT, 8], F32, ss_); cdall_T = T()
                        dte = sb("s_dte", [128, NT, 8], F32, ss_); dte_T = T()
                        ea = sb("s_ea", [128, 8], F32, ss_); ea_T = T()
                        hs = sb("s_hs", [128, 4, 64], F32, ss_); hs_T = T()
                        hbf = sb("s_hbf", [128, 256], BF16, ss_); hbf_T = T()
                        rG = Ring(nc, ss_, "s_G", [128, 128], F32, 2)
                        rR4 = Ring(nc, ss_, "s_r4", [128, 4, 128], F32, 1)
                        rEx = Ring(nc, ss_, "s_ex", [128, 4, 128], F32, 1)
                        rM = Ring(nc, ss_, "s_M", [128, 4, 128], BF16, 2)
                        rXs = Ring(nc, ss_, "s_xs", [128, 4, 64], BF16, 2)
                        rXd = Ring(nc, ss_, "s_xd", [128, 4, 64], BF16, 2)
                        rXd2 = Ring(nc, ss_, "s_xd2", [128, 4, 64], BF16, 2)
                        rBt = Ring(nc, ss_, "s_bt", [128, 128], BF16, 2)
                        rT1 = Ring(nc, ss_, "s_t1", [128, 4, 64], F32, 2)
                        rT3 = Ring(nc, ss_, "s_t3", [128, 4, 64], F32, 1)
                        rSz = Ring(nc, ss_, "s_sz", [128, 256], F32, 3)
                        rYo = Ring(nc, ss_, "s_yo", [128, 256], BF16, 2)
                        rSm = Ring(nc, ss_, "s_sm", [128, 4], F32, 3)
                        junk = sb("s_junk", [128, 256], BF16, ss_); junk_T = T()

                        pdt, pdt_T = PSF[4], PSF_T[4]
                        for i in range(NT):
                            for kc in range(8):
                                op("pe", lambda e, kc=kc, i=i: e.matmul(pdt[:, i * 8:(i + 1) * 8], lhsT=HT[:, kc, i * 128:(i + 1) * 128],
                                                                        rhs=wzz[:, kc, 512:520], start=(kc == 0), stop=(kc == 7)),
                                   r=[HT_T[i], wdt_T], w=[pdt_T])
                        pdt3 = pdt[:, 0:256].rearrange("p (c h) -> p c h", h=8)
                        op("dve", lambda e: e.tensor_tensor(out=dtt[:], in0=pdt3, in1=bc(hv[:, 0:1, :], [128, NT, 8]), op=ALU.add),
                           r=[pdt_T, hv_T], w=[dtt_T])
                        op("act", lambda e: e.activation(out=dtt[:], in_=dtt[:], func=AF.Exp), r=[dtt_T], w=[dtt_T])
                        op("dve", lambda e: e.tensor_scalar(out=dtt[:], in0=dtt[:], scalar1=1.0, scalar2=None, op0=ALU.add),
                           r=[dtt_T], w=[dtt_T])
                        op("act", lambda e: e.activation(out=dtt[:], in_=dtt[:], func=AF.Ln), r=[dtt_T], w=[dtt_T])
                        if layer == 0:
                            stop_at(0.5)
                        op("act", lambda e: e.activation(out=ea[:], in_=hv[:, 1, :], func=AF.Exp), r=[hv_T], w=[ea_T])
                        op("dve", lambda e: e.scalar_tensor_tensor(out=dtA[:], in0=dtt[:], scalar=-1.0,
                                                                   in1=bc(ea[:].rearrange("p (o h) -> p o h", o=1), [128, NT, 8]),
                                                                   op0=ALU.mult, op1=ALU.mult), r=[dtt_T, ea_T], w=[dtA_T])
                        fl = lambda t_: t_[:].rearrange("p c h -> p (c h)")
                        pac, pac_T = PSF[5], PSF_T[5]
                        op("pe", lambda e: e.matmul(pac[:, 0:256], lhsT=LEm, rhs=fl(dtA), start=True, stop=True),
                           r=[cst_T, dtA_T], w=[pac_T])
                        op("pe", lambda e: e.matmul(pac[:, 256:512], lhsT=ones, rhs=fl(dtA), start=True, stop=True),
                           r=[cst_T, dtA_T], w=[pac_T])
                        acopy(fl(acum), pac[:, 0:256], [pac_T], [acum_T])
                        op("act", lambda e: e.activation(out=fl(Eall), in_=pac[:, 0:256], func=AF.Exp), r=[pac_T], w=[Eall_T])
                        op("act", lambda e: e.activation(out=fl(cdall), in_=pac[:, 256:512], func=AF.Exp), r=[pac_T], w=[cdall_T])
                        op("dve", lambda e: e.tensor_tensor(out=fl(dte), in0=pac[:, 256:512], in1=fl(acum), op=ALU.subtract),
                           r=[pac_T, acum_T], w=[dte_T])
                        op("act", lambda e: e.activation(out=fl(dte), in_=fl(dte), func=AF.Exp), r=[dte_T], w=[dte_T])

                        if layer == 0:
                            stop_at(0.6)

                        def hb(t_, c, g):
                            return t_[:, c:c + 1, g * 4:(g + 1) * 4].rearrange("p o h -> p h o")

                        for g in range(2):
                            cb = 1280 + g * 512
                            wqs = load_w(cb, 4)
                            for q in range(4):
                                wq, wq_T = wqs[q]
                                fpad, fpad_T = FB[q % 2], FB_T[q % 2]
                                op("pool", lambda e: e.memset(fpad[:, 0:32], 0.0), w=[fpad_T])
                                for blk in range(8):
                                    pq, pq_T = proj_blk(wq, wq_T, blk)
                                    acopy(fpad[:, 32 + blk * 512:32 + (blk + 1) * 512], pq[:], [pq_T], [fpad_T])
                                ci = g * 4 + q
                                conv_taps(fpad, fpad_T, 4, cws[:, ci, 0:4], cws_T, FB[2], FB_T[2], bias=cws[:, ci, 4:5])
                                for s0 in range(0, L, 2048):
                                    op("act", lambda e, s0=s0, q=q: e.activation(out=HB[q][:, s0:s0 + 2048], in_=FB[2][:, s0:s0 + 2048],
                                                                                func=AF.Silu), r=[FB_T[2]], w=[HB_T[q]])
                            if layer == 0 and g == 0:
                                stop_at(0.7)
                            op("dve", lambda e: e.memset(hs[:], 0.0), w=[hs_T])
                            op("dve", lambda e: e.memset(hbf[:], 0.0), w=[hbf_T])
                            def front(c):
                                sl = slice(c * 128, (c + 1) * 128)
                                op("pe", lambda e: e.matmul(PSF[1][:, 0:128], lhsT=HB[2][:, sl], rhs=HB[3][:, sl], start=True, stop=True),
                                   r=[HB_T[2], HB_T[3]], w=[PSF_T[1]])
                                G, G_T = rG.next()
                                op("dve", lambda e: e.tensor_tensor(out=G[:], in0=PSF[1][:, 0:128], in1=LEm, op=ALU.mult),
                                   r=[PSF_T[1], cst_T], w=[G_T])
                                r4, r4_T = rR4.next()
                                op("dve", lambda e: e.tensor_tensor(out=r4[:], in0=bc(LEm.rearrange("p (o l) -> p o l", o=1), [128, 4, 128]),
                                                                    in1=bc(hb(dtA, c, g), [128, 4, 128]), op=ALU.mult),
                                   r=[cst_T, dtA_T], w=[r4_T])
                                sgi = 0 if c % 2 == 0 else 5
                                op("pe", lambda e: e.matmul(PSF[sgi][:], lhsT=LTs, rhs=r4[:].rearrange("p h l -> p (h l)"),
                                                            start=True, stop=True), r=[cst_T, r4_T], w=[PSF_T[sgi]])
                                ex, ex_T = rEx.next()
                                op("act", lambda e: e.activation(out=ex[:].rearrange("p h l -> p (h l)"), in_=PSF[sgi][:], func=AF.Exp),
                                   r=[PSF_T[sgi]], w=[ex_T])
                                M, M_T = rM.next()
                                op("pool", lambda e: e.tensor_tensor(out=M[:], in0=ex[:],
                                                                     in1=bc(G[:].rearrange("p (o l) -> p o l", o=1), [128, 4, 128]),
                                                                     op=ALU.mult), r=[ex_T, G_T], w=[M_T])
                                for q in range(2):
                                    op("pe", lambda e, q=q: e.transpose(out=PSB[0][:, q * 128:(q + 1) * 128], in_=HB[q][:, sl],
                                                                        identity=identb[:]), r=[HB_T[q], identb_T], w=[PSB_T[0]])
                                xs, xs_T = rXs.next()
                                px3 = PSB[0][:, 0:256].rearrange("p (h d) -> p h d", h=4)
                                acopy(xs[:], px3, [PSB_T[0]], [xs_T])
                                xd, xd_T = rXd.next()
                                op("dve", lambda e: e.tensor_tensor(out=xd[:], in0=px3, in1=bc(hb(dtt, c, g), [128, 4, 64]), op=ALU.mult),
                                   r=[PSB_T[0], dtt_T], w=[xd_T])
                                xd2, xd2_T = rXd2.next()
                                op("pool", lambda e: e.tensor_tensor(out=xd2[:], in0=xd[:], in1=bc(hb(dte, c, g), [128, 4, 64]), op=ALU.mult),
                                   r=[xd_T, dte_T], w=[xd2_T])
                                op("pe", lambda e: e.transpose(out=PSB[0][:, 256:384], in_=HB[2][:, sl], identity=identb[:]),
                                   r=[HB_T[2], identb_T], w=[PSB_T[0]])
                                bt, bt_T = rBt.next()
                                acopy(bt[:], PSB[0][:, 256:384], [PSB_T[0]], [bt_T])
                                for kc in range(8):
                                    op("pe", lambda e, kc=kc: e.matmul(PSF[4][:, 0:256], lhsT=HT[:, kc, sl], rhs=wzz[:, kc, g * 256:(g + 1) * 256],
                                                                       start=(kc == 0), stop=(kc == 7)), r=[HT_T[c], wz_T], w=[PSF_T[4]])
                                sz, sz_T = rSz.next()
                                op("act", lambda e: e.activation(out=sz[:], in_=PSF[4][:, 0:256], func=AF.Exp, scale=-1.0),
                                   r=[PSF_T[4]], w=[sz_T])
                                op("dve", lambda e: e.tensor_scalar(out=sz[:], in0=sz[:], scalar1=1.0, scalar2=None, op0=ALU.add),
                                   r=[sz_T], w=[sz_T])
                                op("dve", lambda e: e.reciprocal(out=sz[:], in_=sz[:]), r=[sz_T], w=[sz_T])
                                op("dve", lambda e: e.tensor_tensor(out=sz[:], in0=sz[:], in1=PSF[4][:, 0:256], op=ALU.mult),
                                   r=[sz_T, PSF_T[4]], w=[sz_T])
                                return (M, M_T, xs, xs_T, xd, xd_T, xd2, xd2_T, bt, bt_T, sz, sz_T)

                            def back(c, P):
                                M, M_T, xs, xs_T, xd, xd_T, xd2, xd2_T, bt, bt_T, sz, sz_T = P
                                sl = slice(c * 128, (c + 1) * 128)
                                for r_ in range(4):
                                    op("pe", lambda e, r_=r_: e.matmul(PSF[2][:, r_ * 64:(r_ + 1) * 64], lhsT=M[:, r_, :], rhs=xd[:, r_, :],
                                                                       start=True, stop=True), r=[M_T, xd_T], w=[PSF_T[2]])
                                op("pe", lambda e: e.matmul(PSF[3][:, 0:256], lhsT=HB[3][:, sl], rhs=hbf[:], start=True, stop=True),
                                   r=[HB_T[3], hbf_T], w=[PSF_T[3]])
                                op("pe", lambda e: e.matmul(PSF[3][:, 256:512], lhsT=bt[:], rhs=xd2[:].rearrange("p h d -> p (h d)"),
                                                            start=True, stop=True), r=[bt_T, xd2_T], w=[PSF_T[3]])
                                t1, t1_T = rT1.next()
                                op("dve", lambda e: e.tensor_tensor(out=t1[:], in0=PSF[3][:, 0:256].rearrange("p (h d) -> p h d", h=4),
                                                                    in1=bc(hb(Eall, c, g), [128, 4, 64]), op=ALU.mult),
                                   r=[PSF_T[3], Eall_T], w=[t1_T])
                                op("dve", lambda e: e.tensor_tensor(out=t1[:], in0=t1[:],
                                                                    in1=PSF[2][:, 0:256].rearrange("p (h d) -> p h d", h=4), op=ALU.add),
                                   r=[t1_T, PSF_T[2]], w=[t1_T])
                                t3, t3_T = rT3.next()
                                op("pool", lambda e: e.tensor_tensor(out=t3[:], in0=xs[:],
                                                                     in1=bc(hv[:, 2:3, g * 4:(g + 1) * 4].rearrange("p o h -> p h o"), [128, 4, 64]),
                                                                     op=ALU.mult), r=[xs_T, hv_T], w=[t3_T])
                                op("dve", lambda e: e.tensor_tensor(out=t1[:], in0=t1[:], in1=t3[:], op=ALU.add), r=[t1_T, t3_T], w=[t1_T])
                                op("dve", lambda e: e.tensor_tensor(out=hs[:], in0=hs[:], in1=bc(hb(cdall, c, g), [128, 4, 64]), op=ALU.mult),
                                   r=[hs_T, cdall_T], w=[hs_T])
                                op("dve", lambda e: e.tensor_tensor(out=hs[:], in0=hs[:],
                                                                    in1=PSF[3][:, 256:512].rearrange("p (h d) -> p h d", h=4), op=ALU.add),
                                   r=[hs_T, PSF_T[3]], w=[hs_T])
                                acopy(hbf[:], hs[:].rearrange("p h d -> p (h d)"), [hs_T], [hbf_T])
                                return (t1, t1_T, sz, sz_T)

                            def back2(c, Q):
                                t1, t1_T, sz, sz_T = Q
                                sl = slice(c * 128, (c + 1) * 128)
                                op("dve", lambda e: e.tensor_tensor(out=sz[:], in0=sz[:], in1=t1[:].rearrange("p h d -> p (h d)"), op=ALU.mult),
                                   r=[sz_T, t1_T], w=[sz_T])
                                sm, sm_T = rSm.next()
                                op("act", lambda e: e.activation(out=junk[:], in_=sz[:], func=AF.Square, accum_out=sm[:, 0:1]),
                                   r=[sz_T], w=[junk_T, sm_T])
                                rstd_from_ss(sm[:, 0:1], 256, sm[:, 1:2], sm[:, 2:3], sm_T)
                                yo, yo_T = rYo.next()
                                op("dve", lambda e: e.scalar_tensor_tensor(out=yo[:], in0=sz[:], scalar=sm[:, 2:3],
                                                                           in1=snw[:, g * 256:(g + 1) * 256], op0=ALU.mult, op1=ALU.mult),
                                   r=[sz_T, sm_T, snw_T], w=[yo_T])
                                for q in range(2):
                                    op("pe", lambda e, q=q: e.transpose(out=PSB[1][:, q * 128:(q + 1) * 128], in_=yo[:, q * 128:(q + 1) * 128],
                                                                        identity=identb[:]), r=[yo_T, identb_T], w=[PSB_T[1]])
                                if c % 2 == 0:
                                    ystate["y"] = rYst.next()
                                yst, yst_T = ystate["y"]
                                acopy(yst[:, :, (c % 2) * 128:(c % 2 + 1) * 128], PSB[1][:, 0:256].rearrange("p (q t) -> p q t", q=2),
                                      [PSB_T[1]], [yst_T])
                                if c % 2 == 1:
                                    for q in range(2):
                                        r0 = 256 + g * 256 + q * 128
                                        dma("sp", yT_d[r0:r0 + 128, (c - 1) * 128:(c + 1) * 128], yst[:, q, :], r=[yst_T],
                                            w=[yT_T[2 + g * 2 + q]] if c == NT - 1 else [T()])

                            ystate = {}
                            P_ = front(0)
                            Qp = None
                            for c in range(NT):
                                Pn = front(c + 1) if c + 1 < NT else None
                                Q_ = back(c, P_)
                                if Qp is not None:
                                    back2(c - 1, Qp)
                                Qp = Q_
                                P_ = Pn
                            back2(NT - 1, Qp)
                        barrier()
                    barrier()
                stop_at(1 + 10 * layer)

                moe = (layer == 1)
                if moe:
                    hstack.close()
                with contextlib.ExitStack() as ps:
                    wo = sb("c_wo", [128, 8, D], BF16, ps); wo_T = T()
                    dma("pool", wo[:], wout_d[layer].rearrange("(cc p) d -> p cc d", p=128), w=[wo_T])
                    ytl = Ring(nc, ps, "c_y", [128, 8, 512], BF16, 2)
                    xin = Ring(nc, ps, "c_x", [128, D], F32, 3)
                    smr = Ring(nc, ps, "c_sm", [128, 4], F32, 3)
                    junk = sb("c_junk", [128, D], BF16, ps); junk_T = T()
                    dma("sp", nwb[:], nw_d[:, 1 + 2 * layer, :], w=[nwb_T])
                    yT_v = yT_d.rearrange("(cc p) t -> p cc t", p=128)
                    if not moe:
                        hnr = Ring(nc, ps, "c_hn", [128, D], BF16, 2)
                    else:
                        hn32 = Ring(nc, ps, "c_h32", [128, D], F32, 2)
                        h3T = Ring(nc, ps, "c_h3T", [128, 8, 128], F32, 2)
                        H3 = sb("m_H3", [128, NT, D], BF16, ps); H3_T = TL(NT, "H3")
                        wr = sb("m_wr", [128, 8, NE], F32, ps); wr_T = T()
                        dma("sp", wr[:], wr_d.rearrange("(kc p) e -> p kc e", p=128), w=[wr_T])
                        lgr = Ring(nc, ps, "m_lg", [128, 24], F32, 3)
                        M1m = sb("m_M1", [128, NT, NE], F32, ps)
                        M2m = sb("m_M2", [128, NT, NE], F32, ps)
                        RK = sb("m_RK", [128, NT, NE], F32, ps)
                        tot = sb("m_tot", [128, NE], F32, ps); tot_T = T()
                        op("dve", lambda e: e.memset(tot[:], 0.0), w=[tot_T])
                    for blk in range(8):
                        yl, yl_T = ytl.next()
                        dma("sp", yl[:], yT_v[:, :, blk * 512:(blk + 1) * 512], r=yT_T, w=[yl_T])
                        for s in range(4):
                            i = blk * 4 + s
                            xt, xt_T = xin.next()
                            dma("sp", xt[:], res_src[i * 128:(i + 1) * 128, :], r=([xres_T[i]] if layer else []), w=[xt_T])
                            for dh in range(2):
                                bi = (2 * i + dh) % 4
                                for cc in range(8):
                                    op("pe", lambda e, cc=cc: e.matmul(PSF[bi][:], lhsT=yl[:, cc, s * 128:(s + 1) * 128],
                                                                       rhs=wo[:, cc, dh * 512:(dh + 1) * 512],
                                                                       start=(cc == 0), stop=(cc == 7)), r=[yl_T, wo_T], w=[PSF_T[bi]])
                                op("dve", lambda e: e.tensor_tensor(out=xt[:, dh * 512:(dh + 1) * 512], in0=xt[:, dh * 512:(dh + 1) * 512],
                                                                    in1=PSF[bi][:], op=ALU.add), r=[xt_T, PSF_T[bi]], w=[xt_T])
                            dma("sp", xres_d[i * 128:(i + 1) * 128, :], xt[:], r=[xt_T], w=[xres_T[i]])
                            if not moe:
                                hn, hn_T = hnr.next()
                                norm_tile(xt[:], xt_T, hn[:], hn_T, smr, junk, junk_T)
                                to_HT(hn, hn_T, i)
                            else:
                                h32, h32_T = hn32.next()
                                norm_tile(xt[:], xt_T, h32[:], h32_T, smr, junk, junk_T)
                                acopy(H3[:, i, :], h32[:], [h32_T], [H3_T[i]], eng="pool")
                                for k in range(8):
                                    pf = PSF[4 + k // 4]
                                    op("pe", lambda e, k=k: e.transpose(out=pf[:, (k % 4) * 128:(k % 4 + 1) * 128],
                                                                        in_=h32[:, k * 128:(k + 1) * 128], identity=ident),
                                       r=[h32_T, cst_T], w=[PSF_T[4 + k // 4]])
                                hT3, hT3_T = h3T.next()
                                acopy(hT3[:, 0:4, :], PSF[4][:].rearrange("p (k t) -> p k t", k=4), [PSF_T[4]], [hT3_T])
                                acopy(hT3[:, 4:8, :], PSF[5][:].rearrange("p (k t) -> p k t", k=4), [PSF_T[5]], [hT3_T], eng="dve")
                                pl, pl_T = PSB[0], PSB_T[0]
                                plf = PSF[(2 * i + 2) % 4]
                                plf_T = PSF_T[(2 * i + 2) % 4]
                                for kc in range(8):
                                    op("pe", lambda e, kc=kc: e.matmul(plf[:, 0:NE], lhsT=hT3[:, kc, :], rhs=wr[:, kc, :],
                                                                       start=(kc == 0), stop=(kc == 7)), r=[hT3_T, wr_T], w=[plf_T])
                                lg, lg_T = lgr.next()
                                acopy(lg[:, 0:8], plf[:, 0:NE], [plf_T], [lg_T])
                                dv = lambda fn, r=(), w=(): op("dve", fn, r=list(r), w=list(w))
                                dv(lambda e: e.max(out=lg[:, 8:16], in_=lg[:, 0:8]), [lg_T], [lg_T])
                                dv(lambda e: e.tensor_scalar(out=M1m[:, i, :], in0=lg[:, 0:8], scalar1=lg[:, 8:9], scalar2=None,
                                                             op0=ALU.is_ge), [lg_T], [rt_T])
                                dv(lambda e: e.tensor_scalar(out=lg[:, 16:24], in0=lg[:, 0:8], scalar1=lg[:, 9:10], scalar2=None,
                                                             op0=ALU.is_ge), [lg_T], [lg_T])
                                dv(lambda e: e.tensor_tensor(out=M2m[:, i, :], in0=lg[:, 16:24], in1=M1m[:, i, :], op=ALU.subtract),
                                   [lg_T, rt_T], [rt_T])
                                dv(lambda e: e.tensor_tensor(out=lg[:, 10:11], in0=lg[:, 9:10], in1=lg[:, 8:9], op=ALU.subtract),
                                   [lg_T], [lg_T])
                                op("act", lambda e: e.activation(out=lg[:, 10:11], in_=lg[:, 10:11], func=AF.Exp), r=[lg_T], w=[lg_T])
                                dv(lambda e: e.tensor_scalar(out=lg[:, 11:12], in0=lg[:, 10:11], scalar1=1.0, scalar2=None, op0=ALU.add),
                                   [lg_T], [lg_T])
                                dv(lambda e: e.reciprocal(out=GT[:, i, 0:1], in_=lg[:, 11:12]), [lg_T], [rt_T])
                                dv(lambda e: e.tensor_tensor(out=GT[:, i, 1:2], in0=lg[:, 10:11], in1=GT[:, i, 0:1], op=ALU.mult),
                                   [lg_T, rt_T], [rt_T])
                                prk = PSF[(2 * i + 3) % 4]
                                prk_T = PSF_T[(2 * i + 3) % 4]
                                op("pe", lambda e: e.matmul(prk[:, 0:8], lhsT=SUm, rhs=lg[:, 16:24], start=True, stop=True),
                                   r=[cst_T, lg_T], w=[prk_T])
                                op("pe", lambda e: e.matmul(prk[:, 8:16], lhsT=ones, rhs=lg[:, 16:24], start=True, stop=True),
                                   r=[cst_T, lg_T], w=[prk_T])
                                dv(lambda e: e.tensor_tensor(out=RK[:, i, :], in0=prk[:, 0:8], in1=tot[:], op=ALU.add),
                                   [prk_T, tot_T], [rt_T])
                                dv(lambda e: e.tensor_tensor(out=tot[:], in0=tot[:], in1=prk[:, 8:16], op=ALU.add),
                                   [prk_T, tot_T], [tot_T])
                    if moe:
                        zt = sb("m_zero", [128, 8, D], BF16, ps); zt_T = T()
                        op("pool", lambda e: e.memset(zt[:], 0.0), w=[zt_T])
                        hmz_T = T("hmz")
                        ybz_T = T("ybz")
                        hm_v = hm_d.rearrange("(b p) d -> p b d", p=128)
                        for b0 in range(0, NBLK + 1, 8):
                            nb = min(8, NBLK + 1 - b0)
                            dma("sp", hm_v[:, b0:b0 + nb, :], zt[:, 0:nb, :], r=[zt_T], w=[hmz_T] if b0 == 0 else [T()])
                        hmz_all = hmz_T
                        zf = sb("m_zf", [128, D], F32, ps); zf_T = T()
                        op("pool", lambda e: e.memset(zf[:], 0.0), w=[zf_T])
                        dma("sp", yb_d[NROW:NROW + 128, :], zf[:], r=[zf_T], w=[ybz_T])
                        RS = sb("m_RS", [128, NT, NE], F32, ps)
                        prod = sb("m_prod", [128, NT, NE], F32, ps)
                        dstf = sb("m_dstf", [128, 2, NT], F32, ps)
                        rkf = sb("m_rkf", [128, NT], F32, ps)
                        stv = sb("m_stv", [128, NE], F32, ps)
                        for e_ in range(NE):
                            dv(lambda e, e_=e_: e.memset(stv[:, e_:e_ + 1], float(e_ * CAPROWS)), [], [rt_T])
                        dv(lambda e: e.tensor_tensor(out=RS[:], in0=RK[:], in1=bc(stv[:].rearrange("p (o e) -> p o e", o=1), [128, NT, NE]),
                                                     op=ALU.add), [rt_T], [rt_T])
                        for k, Mk in enumerate((M1m, M2m)):
                            dv(lambda e, Mk=Mk: e.tensor_tensor(out=prod[:], in0=RS[:], in1=Mk[:], op=ALU.mult), [rt_T], [rt_T])
                            dv(lambda e, k=k: e.tensor_reduce(out=dstf[:, k, :], in_=prod[:], axis=AX.X, op=ALU.add), [rt_T], [rt_T])
                            dv(lambda e, Mk=Mk: e.tensor_tensor(out=prod[:], in0=RK[:], in1=Mk[:], op=ALU.mult), [rt_T], [rt_T])
                            dv(lambda e: e.tensor_reduce(out=rkf[:], in_=prod[:], axis=AX.X, op=ALU.add), [rt_T], [rt_T])
                            dv(lambda e: e.tensor_scalar(out=rkf[:], in0=rkf[:], scalar1=float(CAPROWS), scalar2=1.0e6,
                                                         op0=ALU.is_ge, op1=ALU.mult), [rt_T], [rt_T])
                            dv(lambda e, k=k: e.tensor_tensor(out=dstf[:, k, :], in0=dstf[:, k, :], in1=rkf[:], op=ALU.add), [rt_T], [rt_T])
                            dv(lambda e, k=k: e.tensor_scalar(out=dstf[:, k, :], in0=dstf[:, k, :], scalar1=float(NROW), scalar2=None,
                                                              op0=ALU.min), [rt_T], [rt_T])
                        dv(lambda e: e.tensor_copy(out=desti[:], in_=dstf[:]), [rt_T], [rt_T])
                        hms_T = TL(2 * NT, "hms")
                        barrier()
                        for i in range(NT):
                            for k in range(2):
                                dma("pool", hm_d[:, :], H3[:, i, :], r=[H3_T[i], rt_T], w=[hms_T[2 * i + k]],
                                    indirect=dict(out_offset=bass.IndirectOffsetOnAxis(ap=desti[:, k, i:i + 1], axis=0), in_offset=None))
                    barrier()
                stop_at(2 + 10 * layer)

                if not moe:
                    with contextlib.ExitStack() as ps:
                        wd = sb("d_wd", [128, NFF, D], BF16, ps); wd_T = T()
                        dma("pool", wd[:], ffd_d.rearrange("(j p) d -> p j d", p=128), w=[wd_T])
                        aT = sb("d_aT", [128, NFF, 512], BF16, ps); aT_T = TL(NFF, "aT")
                        wgu = Ring(nc, ps, "d_wgu", [128, 2, 8, 256], BF16, 3)
                        sgr = Ring(nc, ps, "d_sg", [128, 512], F32, 2)
                        xin = Ring(nc, ps, "d_x", [128, D], F32, 3)
                        hnr = Ring(nc, ps, "d_hn", [128, D], BF16, 2)
                        smr = Ring(nc, ps, "d_sm", [128, 4], F32, 3)
                        junk = sb("d_junk", [128, D], BF16, ps); junk_T = T()
                        dma("sp", nwb[:], nw_d[:, 2, :], w=[nwb_T])
                        ffg_v = ffg_d.rearrange("(kc p) f -> p kc f", p=128)
                        ffu_v = ffu_d.rearrange("(kc p) f -> p kc f", p=128)
                        for tb in range(8):
                            tsl = slice(tb * 512, (tb + 1) * 512)
                            for j in range(NFF):
                                if j % 2 == 0:
                                    wt2, wt_T = wgu.next()
                                    dma("pool", wt2[:, 0], ffg_v[:, :, j * 128:(j + 2) * 128], w=[wt_T])
                                    dma("pool", wt2[:, 1], ffu_v[:, :, j * 128:(j + 2) * 128], w=[wt_T])
                                wt = wt2[:, :, :, (j % 2) * 128:(j % 2 + 1) * 128]
                                pg, pg_T = PSF[j % 2], PSF_T[j % 2]
                                pu, pu_T = PSF[2 + j % 2], PSF_T[2 + j % 2]
                                rT = [wt_T] + HT_T[tb * 4:(tb + 1) * 4]
                                for kc in range(8):
                                    op("pe", lambda e, kc=kc: e.matmul(pg[:], lhsT=wt[:, 0, kc, :], rhs=HT[:, kc, tsl],
                                                                       start=(kc == 0), stop=(kc == 7)), r=rT, w=[pg_T])
                                for kc in range(8):
                                    op("pe", lambda e, kc=kc: e.matmul(pu[:], lhsT=wt[:, 1, kc, :], rhs=HT[:, kc, tsl],
                                                                       start=(kc == 0), stop=(kc == 7)), r=rT, w=[pu_T])
                                sg, sg_T = sgr.next()
                                op("act", lambda e: e.activation(out=sg[:], in_=pg[:], func=AF.Silu), r=[pg_T], w=[sg_T])
                                op("dve", lambda e, j=j: e.tensor_tensor(out=aT[:, j, :], in0=sg[:], in1=pu[:], op=ALU.mult),
                                   r=[sg_T, pu_T], w=[aT_T[j]])
                            for s in range(4):
                                i = tb * 4 + s
                                xt, xt_T = xin.next()
                                dma("sp", xt[:], xres_d[i * 128:(i + 1) * 128, :], r=[xres_T[i]], w=[xt_T])
                                for dh in range(2):
                                    pd, pd_T = PSF[4 + dh], PSF_T[4 + dh]
                                    for j in range(NFF):
                                        op("pe", lambda e, j=j: e.matmul(pd[:], lhsT=aT[:, j, s * 128:(s + 1) * 128],
                                                                         rhs=wd[:, j, dh * 512:(dh + 1) * 512],
                                                                         start=(j == 0), stop=(j == NFF - 1)), r=[aT_T[j], wd_T], w=[pd_T])
                                    op("dve", lambda e: e.tensor_tensor(out=xt[:, dh * 512:(dh + 1) * 512], in0=xt[:, dh * 512:(dh + 1) * 512],
                                                                        in1=pd[:], op=ALU.add), r=[xt_T, pd_T], w=[xt_T])
                                dma("sp", xres_d[i * 128:(i + 1) * 128, :], xt[:], r=[xt_T], w=[xres_T[i]])
                                hn, hn_T = hnr.next()
                                norm_tile(xt[:], xt_T, hn[:], hn_T, smr, junk, junk_T)
                                to_HT(hn, hn_T, i)
                        barrier()
                else:
                    with contextlib.ExitStack() as ps:
                        XT = sb("e_XT", [128, CAP, 8, 128], BF16, ps); XT_T = TL(CAP, "XT")
                        aT = sb("e_aT", [128, NFE, CAP * 128], BF16, ps); aT_T = TL(NFE, "eaT")
                        xb = Ring(nc, ps, "e_xb", [128, D], BF16, 3)
                        wgu = Ring(nc, ps, "e_wgu", [128, 2, 8, 256], BF16, 4)
                        wdq = Ring(nc, ps, "e_wd", [128, NFE, 256], BF16, 2)
                        sgr = Ring(nc, ps, "e_sg", [128, 512], F32, 2)
                        yor = Ring(nc, ps, "e_yo", [128, 512], F32, 3)
                        yb_T = TL(NBLK, "yb")
                        for ex_ in range(NE):
                            for bb in range(CAP):
                                b = ex_ * CAP + bb
                                xt, xt_T = xb.next()
                                dma("sp", xt[:], hm_d[b * 128:(b + 1) * 128, :], r=[hmz_all] + hms_T, w=[xt_T])
                                pb, pb_T = PSB[bb % 2], PSB_T[bb % 2]
                                for k in range(8):
                                    op("pe", lambda e, k=k: e.transpose(out=pb[:, k * 128:(k + 1) * 128], in_=xt[:, k * 128:(k + 1) * 128],
                                                                        identity=identb[:]), r=[xt_T, identb_T], w=[pb_T])
                                acopy(XT[:, bb, :, :], pb[:].rearrange("p (k t) -> p k t", k=8), [pb_T], [XT_T[bb]],
                                      eng=("act" if bb % 2 == 0 else "dve"))
                            mg_v = mg_d[ex_].rearrange("(kc p) f -> p kc f", p=128)
                            mu_v = mu_d[ex_].rearrange("(kc p) f -> p kc f", p=128)
                            md_v = md_d[ex_].rearrange("(j p) d -> p j d", p=128)
                            for j in range(NFE):
                                if j % 2 == 0:
                                    wt2, wt_T = wgu.next()
                                    dma("pool", wt2[:, 0], mg_v[:, :, j * 128:(j + 2) * 128], w=[wt_T])
                                    dma("pool", wt2[:, 1], mu_v[:, :, j * 128:(j + 2) * 128], w=[wt_T])
                                wt = wt2[:, :, :, (j % 2) * 128:(j % 2 + 1) * 128]
                                for qd in range(CAP // 4):
                                    pg, pg_T = PSF[qd % 2], PSF_T[qd % 2]
                                    pu, pu_T = PSF[2 + qd % 2], PSF_T[2 + qd % 2]
                                    rT = [wt_T] + XT_T[qd * 4:(qd + 1) * 4]
                                    for gu, pp in ((0, pg), (1, pu)):
                                        for kc in range(8):
                                            op("pe", lambda e, kc=kc, gu=gu, pp=pp: e.matmul(
                                                pp[:].rearrange("p (b t) -> p b t", b=4), lhsT=wt[:, gu, kc, :],
                                                rhs=XT[:, qd * 4:(qd + 1) * 4, kc, :], start=(kc == 0), stop=(kc == 7)),
                                               r=rT, w=[pg_T if gu == 0 else pu_T])
                                    sg, sg_T = sgr.next()
                                    op("act", lambda e: e.activation(out=sg[:], in_=pg[:], func=AF.Silu), r=[pg_T], w=[sg_T])
                                    op("dve", lambda e, j=j, qd=qd: e.tensor_tensor(out=aT[:, j, qd * 512:(qd + 1) * 512], in0=sg[:],
                                                                                     in1=pu[:], op=ALU.mult),
                                       r=[sg_T, pu_T], w=[aT_T[j]])
                            for dq in range(4):
                                wq, wq_T = wdq.next()
                                dma("pool", wq[:], md_v[:, :, dq * 256:(dq + 1) * 256], w=[wq_T])
                                for b2 in range(CAP // 2):
                                    pd, pd_T = PSF[4 + b2 % 2], PSF_T[4 + b2 % 2]
                                    for u in range(2):
                                        bb = b2 * 2 + u
                                        for j in range(NFE):
                                            op("pe", lambda e, j=j, u=u, bb=bb: e.matmul(
                                                pd[:, u * 256:(u + 1) * 256], lhsT=aT[:, j, bb * 128:(bb + 1) * 128], rhs=wq[:, j, :],
                                                start=(j == 0), stop=(j == NFE - 1)), r=[aT_T[j], wq_T], w=[pd_T])
                                    yo, yo_T = yor.next()
                                    acopy(yo[:], pd[:], [pd_T], [yo_T], eng=("act" if b2 % 2 == 0 else "dve"))
                                    for u in range(2):
                                        b = ex_ * CAP + b2 * 2 + u
                                        dma("sp", yb_d[b * 128:(b + 1) * 128, dq * 256:(dq + 1) * 256], yo[:, u * 256:(u + 1) * 256],
                                            r=[yo_T], w=[yb_T[b]] if dq == 3 else [T()])
                        barrier()
                    with contextlib.ExitStack() as ps:
                        xin = Ring(nc, ps, "f_x", [128, D], F32, 3)
                        y0r = Ring(nc, ps, "f_y0", [128, D], F32, 2)
                        y1r = Ring(nc, ps, "f_y1", [128, D], F32, 2)
                        outr = Ring(nc, ps, "f_o", [128, D], F32, 2)
                        smr = Ring(nc, ps, "f_sm", [128, 4], F32, 3)
                        junk = sb("f_junk", [128, D], BF16, ps); junk_T = T()
                        dma("sp", nwb[:], nw_d[:, 4, :], w=[nwb_T])
                        for i in range(NT):
                            xt, xt_T = xin.next()
                            dma("sp", xt[:], xres_d[i * 128:(i + 1) * 128, :], r=[xres_T[i]], w=[xt_T])
                            ys = []
                            for k, rr in enumerate((y0r, y1r)):
                                yk, yk_T = rr.next()
                                dma("pool", yk[:], yb_d[:, :], r=yb_T + [ybz_T, rt_T], w=[yk_T],
                                    indirect=dict(out_offset=None, in_offset=bass.IndirectOffsetOnAxis(ap=desti[:, k, i:i + 1], axis=0)))
                                ys.append((yk, yk_T))
                            for k in range(2):
                                yk, yk_T = ys[k]
                                op("dve", lambda e, k=k, yk=yk: e.scalar_tensor_tensor(out=xt[:], in0=yk[:], scalar=GT[:, i, k:k + 1], in1=xt[:],
                                                                                      op0=ALU.mult, op1=ALU.add),
                                   r=[yk_T, xt_T, rt_T], w=[xt_T])
                            ot, ot_T = outr.next()
                            norm_tile(xt[:], xt_T, ot[:], ot_T, smr, junk, junk_T)
                            dma("sp", out_d[i * 128:(i + 1) * 128, :], ot[:], r=[ot_T], w=[T()])
                        barrier()
        except _Stop:
            pass
        S.finish()
        hstack.close()
        print("ops", S.nop, "waits", S.nwait)
    return nc


def _consts():
    c = np.zeros((128, 768), np.float32)
    i = np.arange(128)
    c[:, 0:128] = np.eye(128)
    c[:, 128:256] = (i[:, None] <= i[None, :])
    c[:, 256:384] = (i[None, :] < i[:, None])
    c[:, 384:512] = (i[:, None] < i[None, :])
    c[:, 512:640] = 1.0
    c[:, 640:768] = 1.0 / 256.0
    return c


def _win_perm():
    A_B, A_C, A_X, S_Z, XBC, DT, GLU = 0, 256, 512, 768, 1280, 2304, 2312
    cols = []
    for j in range(2):
        for base in (A_B, A_C, A_X):
            cols += list(range(base + j * 128, base + (j + 1) * 128))
    for j in range(2):
        cols += list(range(GLU + j * 128, GLU + (j + 1) * 128))
        cols += list(range(GLU + 256 + j * 128, GLU + 256 + (j + 1) * 128))
    for g in range(2):
        cols += list(range(XBC + g * 256, XBC + (g + 1) * 256))
        cols += list(range(XBC + 512 + g * 128, XBC + 512 + (g + 1) * 128))
        cols += list(range(XBC + 768 + g * 128, XBC + 768 + (g + 1) * 128))
    cols += list(range(S_Z, S_Z + 512))
    cols += list(range(DT, DT + 8))
    assert len(cols) == IN_COLS and len(set(cols)) == IN_COLS
    return np.array(cols)


def _prep(inp):
    f = lambda a: np.ascontiguousarray(np.asarray(a, dtype=np.float32))
    bcast = lambda v: np.broadcast_to(v, (128,) + v.shape)
    perm = _win_perm()
    d = {}
    d["cst"] = _consts()
    nw = np.stack([inp["norm_mix"][0], inp["norm_ffn"][0], inp["norm_mix"][1], inp["norm_ffn"][1], inp["norm_final"]])
    d["nw"] = f(bcast(nw))
    for l in range(2):
        d[f"win{l}"] = f(inp["w_in"][l][:, perm])
    cwa = np.transpose(inp["conv_a_w"].reshape(2, 3, 2, 128), (3, 0, 2, 1))
    d["cwa"] = f(cwa)
    sw = np.concatenate([inp["conv_ssd_w"], inp["conv_ssd_b"][:, None, :]], axis=1)
    ch = []
    for g in range(2):
        ch += [g * 256 + np.arange(128), g * 256 + 128 + np.arange(128), 512 + g * 128 + np.arange(128), 768 + g * 128 + np.arange(128)]
    ch = np.stack(ch)
    d["cws"] = f(np.transpose(sw[:, :, ch], (3, 0, 2, 1)))
    cc = np.concatenate([inp["conv_conf_w"], inp["conv_conf_b"][:, None, :], inp["conf_ln_g"][:, None, :],
                         inp["conf_ln_b"][:, None, :]], axis=1)
    d["cwc"] = f(np.transpose(cc.reshape(2, 34, 2, 128), (3, 0, 2, 1)))
    hv = np.stack([inp["dt_bias"], inp["a_log"], inp["d_skip"]], axis=1)
    d["hv"] = f(bcast(hv))
    d["snw"] = f(bcast(inp["ssd_norm_w"]))
    d["wout"] = f(inp["w_out"])
    d["ffg"] = f(inp["ffn_w_gate"][0])
    d["ffu"] = f(inp["ffn_w_up"][0])
    d["ffd"] = f(inp["ffn_w_down"][0])
    d["wr"] = f(inp["moe_router"][0])
    d["mg"] = f(inp["moe_w_gate"][0])
    d["mu"] = f(inp["moe_w_up"][0])
    d["md"] = f(inp["moe_w_down"][0])
    return d


_NC = {}


def kernel(**inputs):
    inp = {k: np.asarray(v) for k, v in inputs.items()}
    if "nc" not in _NC:
        _NC["nc"] = build()
    nc = _NC["nc"]
    shared = _prep(inp)
    x = np.ascontiguousarray(inp["x"], dtype=np.float32)
    in_maps = [dict(shared, x=x[b]) for b in range(8)]
    res = run_bass_kernel_spmd(nc, in_maps, core_ids=list(range(8)))
    return np.stack([np.asarray(r["out"]) for r in res.results]).astype(np.float32)
```

```python
import contextlib
import numpy as np
import concourse.bass as bass
import concourse.mybir as mybir
from concourse.bass_utils import run_bass_kernel_spmd

F32 = mybir.dt.float32
BF16 = mybir.dt.bfloat16
I32 = mybir.dt.int32
U32 = mybir.dt.uint32
AF = mybir.ActivationFunctionType
ALU = mybir.AluOpType
AX = mybir.AxisListType
PE_ENG = mybir.EngineType.PE

L = 4096
D = 1024
NT = L // 128
EPS = 1e-5
IN_COLS = 2824
DFF = 2816
NFF = DFF // 128
NE = 8
DFE = 3584
NFE = DFE // 128


class T:
    __slots__ = ("name", "w", "r", "excl")

    def __init__(self, name="", excl=False):
        self.name = name
        self.w = None
        self.r = {}
        self.excl = excl


def TL(n, name=""):
    return [T(f"{name}{i}") for i in range(n)]


class AS:
    def __init__(self, nc, es):
        self.nc = nc
        self.eng = {"pe": nc.tensor, "act": nc.scalar, "dve": nc.vector, "pool": nc.gpsimd, "sp": nc.sync}
        self.sem = {k: es.enter_context(nc.semaphore("s_" + k)) for k in ("pe", "act", "dve", "pool")}
        self.cnt = {k: 0 for k in self.sem}
        self.dq = {}
        for q, n in (("sp", 16), ("pool", 12), ("act", 6)):
            self.dq[q] = {"sems": [es.enter_context(nc.semaphore(f"d_{q}{i}")) for i in range(n)],
                          "cnt": [0] * n, "next": 0}
        self.known = {k: {} for k in self.eng}
        self.snap = {}
        self.nwait = 0
        self.nop = 0
        self.halt = False

    def _semof(self, key):
        if isinstance(key, tuple):
            return self.dq[key[0]]["sems"][key[1]]
        return self.sem[key]

    def _need(self, eng, deps):
        if self.halt:
            return
        kn = self.known[eng]
        for key, val in deps.items():
            if kn.get(key, 0) >= val:
                continue
            if key == eng and eng == "pe":
                continue
            self.eng[eng].wait_ge(self._semof(key), val)
            self.nwait += 1
            kn[key] = val
            sn = self.snap.get((key, val))
            if sn:
                for k2, v2 in sn.items():
                    if kn.get(k2, 0) < v2:
                        kn[k2] = v2

    @staticmethod
    def _deps(reads, writes, eng=None):
        deps = {}

        def add(kv):
            k, v = kv
            if deps.get(k, 0) < v:
                deps[k] = v
        for t in reads:
            if t.w:
                add(t.w)
            if t.excl:
                for kv in t.r.items():
                    if kv[0] != eng:
                        add(kv)
        for t in writes:
            if t.w:
                add(t.w)
            for kv in t.r.items():
                add(kv)
        return deps

    def op(self, eng, fn, r=(), w=()):
        if self.halt:
            return None
        self._need(eng, self._deps(r, w, eng))
        inst = fn(self.eng[eng])
        self.cnt[eng] += 1
        c = self.cnt[eng]
        inst.then_inc(self.sem[eng], 1)
        self.nop += 1
        self.snap[(eng, c)] = dict(self.known[eng])
        for t in r:
            t.r[eng] = c
        for t in w:
            t.w = (eng, c)
            t.r = {}
        return inst

    def dma(self, q, out, in_, r=(), w=(), indirect=None):
        if self.halt:
            return None
        dq = self.dq[q]
        i = dq["next"]
        dq["next"] = (i + 1) % len(dq["sems"])
        key = (q, i)
        deps = self._deps(r, w)
        if dq["cnt"][i] > 0:
            deps[key] = max(deps.get(key, 0), dq["cnt"][i])
        self._need(q, deps)
        e = self.eng[q]
        if indirect is None:
            inst = e.dma_start(out=out, in_=in_)
        else:
            inst = e.indirect_dma_start(out=out, in_=in_, **indirect)
        dq["cnt"][i] += 16
        v = dq["cnt"][i]
        inst.then_inc(dq["sems"][i], 16)
        self.nop += 1
        self.snap[(key, v)] = dict(self.known[q])
        for t in r:
            t.r[key] = v
        for t in w:
            t.w = (key, v)
            t.r = {}
        return inst

    def finish(self):
        self.halt = False
        deps = {}
        for q, dq in self.dq.items():
            for i, c in enumerate(dq["cnt"]):
                if c > 0:
                    deps[(q, i)] = c
        self._need("sp", deps)
        self._need("sp", {k: c for k, c in self.cnt.items() if c > 0})


_UID = [0]


class Ring:
    def __init__(self, nc, es, name, shape, dtype, n):
        _UID[0] += 1
        self.bufs = [es.enter_context(nc.sbuf_tensor(f"r{_UID[0]}_{name}{i}", shape, dtype)) for i in range(n)]
        self.ts = TL(n, name)
        self.i = 0

    def next(self):
        i = self.i
        self.i = (i + 1) % len(self.bufs)
        return self.bufs[i], self.ts[i]


def bc(ap, shape):
    return ap.to_broadcast(shape)


CAP = 12
CAPROWS = CAP * 128
NBLK = NE * CAP
NROW = NBLK * 128


class _Stop(Exception):
    pass


def build(stop_after=99, debug=False):
    nc = bass.Bass("TRN2", target_bir_lowering=False)

    def din(name, shape, dt=F32):
        return nc.dram_tensor(name, shape, dt, kind="ExternalInput").ap()

    def dscr(name, shape, dt=F32):
        kind = "ExternalOutput" if debug else "Internal"
        return nc.dram_tensor(name, shape, dt, kind=kind).ap()

    x_in = din("x", [L, D])
    cst_d = din("cst", [128, 768])
    nw_d = din("nw", [128, 5, D])
    win_d = [din("win0", [D, IN_COLS]), din("win1", [D, IN_COLS])]
    cwa_d = din("cwa", [128, 2, 2, 3])
    cws_d = din("cws", [128, 2, 8, 5])
    cwc_d = din("cwc", [128, 2, 2, 34])
    hv_d = din("hv", [128, 2, 3, 8])
    snw_d = din("snw", [128, 2, 512])
    wout_d = din("wout", [2, D, D])
    ffg_d = din("ffg", [D, DFF])
    ffu_d = din("ffu", [D, DFF])
    ffd_d = din("ffd", [DFF, D])
    wr_d = din("wr", [D, NE])
    need_moe = stop_after > 12
    if need_moe:
        mg_d = din("mg", [NE, D, DFE])
        mu_d = din("mu", [NE, D, DFE])
        md_d = din("md", [NE, DFE, D])
    else:
        mg_d = nc.dram_tensor("mg", [NE, D, DFE], F32, kind="Internal").ap()
        mu_d = nc.dram_tensor("mu", [NE, D, DFE], F32, kind="Internal").ap()
        md_d = nc.dram_tensor("md", [NE, DFE, D], F32, kind="Internal").ap()
    out_d = nc.dram_tensor("out", [L, D], F32, kind="ExternalOutput").ap()

    xres_d = dscr("xres", [L, D])
    yT_d = dscr("yT", [D, L], BF16)
    hm_d = nc.dram_tensor("hm", [NROW + 128, D], BF16, kind="Internal").ap()
    yb_d = nc.dram_tensor("yb", [NROW + 128, D], F32, kind="Internal").ap()

    xres_T = TL(NT, "xres")
    yT_T = TL(8, "yT")

    with contextlib.ExitStack() as es:
        S = AS(nc, es)
        op, dma = S.op, S.dma

        def sb(name, shape, dt=F32, stack=es):
            _UID[0] += 1
            return stack.enter_context(nc.sbuf_tensor(f"t{_UID[0]}_{name}", shape, dt))

        cst = sb("cst", [128, 768]); cst_T = T("cst")
        identb = sb("identb", [128, 128], BF16); identb_T = T("identb")
        nwb = sb("nwb", [128, D]); nwb_T = T("nwb")
        epsc = sb("epsc", [128, 1]); epsc_T = T("epsc")
        PSF = [es.enter_context(nc.psum_tensor(f"psf{i}", [128, 512], F32)) for i in range(6)]
        PSB = [es.enter_context(nc.psum_tensor(f"psb{i}", [128, 1024], BF16)) for i in range(2)]
        PSF_T = [T(f"psf{i}", excl=True) for i in range(6)]
        PSB_T = [T(f"psb{i}", excl=True) for i in range(2)]
        GT = sb("m_GT", [128, NT, 2], F32)
        desti = sb("m_desti", [128, 2, NT], U32)
        rt_T = T("route")
        hstack = contextlib.ExitStack()
        HT = sb("HT", [128, 8, L], BF16, hstack); HT_T = TL(NT, "HT")

        ident = cst[:, 0:128]
        LEm = cst[:, 128:256]
        LTs = cst[:, 256:384]
        SUm = cst[:, 384:512]
        ones = cst[:, 512:640]
        ones256 = cst[:, 640:768]

        op("dve", lambda e: e.memset(epsc[:], EPS), w=[epsc_T])
        dma("sp", cst[:], cst_d[:], w=[cst_T])
        op("dve", lambda e: e.tensor_copy(out=identb[:], in_=ident), r=[cst_T], w=[identb_T])

        def acopy(out, in_, r, w, eng="act"):
            if eng == "act":
                op("act", lambda e: e.activation(out=out, in_=in_, func=AF.Copy), r=r, w=w)
            else:
                op(eng, lambda e: e.tensor_copy(out=out, in_=in_), r=r, w=w)

        def barrier():
            allc = {k: c for k, c in S.cnt.items() if c > 0}
            for q, dq in S.dq.items():
                for i, c in enumerate(dq["cnt"]):
                    if c > 0:
                        allc[(q, i)] = c
            for e_ in ("pe", "act", "dve", "pool", "sp"):
                S._need(e_, allc)

        def rstd_from_ss(ss, n, tmp, rs, t_):
            op("act", lambda e: e.activation(out=tmp, in_=ss, func=AF.Ln, scale=1.0 / n, bias=epsc[:, 0:1]),
               r=[t_, epsc_T], w=[t_])
            op("act", lambda e: e.activation(out=rs, in_=tmp, func=AF.Exp, scale=-0.5), r=[t_], w=[t_])

        def norm_tile(xt, xt_T, out, out_T, smr, junk, junk_T):
            sm, sm_T = smr.next()
            op("act", lambda e: e.activation(out=junk[:], in_=xt, func=AF.Square, accum_out=sm[:, 0:1]),
               r=[xt_T], w=[junk_T, sm_T])
            rstd_from_ss(sm[:, 0:1], D, sm[:, 1:2], sm[:, 2:3], sm_T)
            op("dve", lambda e: e.scalar_tensor_tensor(out=out, in0=xt, scalar=sm[:, 2:3], in1=nwb[:],
                                                       op0=ALU.mult, op1=ALU.mult),
               r=[xt_T, sm_T, nwb_T], w=[out_T])

        def to_HT(hn, hn_T, i):
            pb, pb_T = PSB[i % 2], PSB_T[i % 2]
            for k in range(8):
                op("pe", lambda e, k=k: e.transpose(out=pb[:, k * 128:(k + 1) * 128], in_=hn[:, k * 128:(k + 1) * 128],
                                                    identity=identb[:]),
                   r=[hn_T, identb_T], w=[pb_T])
            acopy(HT[:, :, i * 128:(i + 1) * 128], pb[:].rearrange("p (k t) -> p k t", k=8), [pb_T], [HT_T[i]])

        def stop_at(x):
            if stop_after <= x:
                S.halt = True

        try:
            for layer in range(2):
                res_src = x_in if layer == 0 else xres_d
                if layer == 0:
                    with contextlib.ExitStack() as ps:
                        xin = Ring(nc, ps, "a_x", [128, D], F32, 3)
                        hnr = Ring(nc, ps, "a_hn", [128, D], BF16, 2)
                        smr = Ring(nc, ps, "a_sm", [128, 4], F32, 3)
                        junk = sb("a_junk", [128, D], BF16, ps); junk_T = T()
                        dma("sp", nwb[:], nw_d[:, 0, :], w=[nwb_T])
                        for i in range(NT):
                            xt, xt_T = xin.next()
                            dma("sp", xt[:], x_in[i * 128:(i + 1) * 128, :], w=[xt_T])
                            hn, hn_T = hnr.next()
                            norm_tile(xt[:], xt_T, hn[:], hn_T, smr, junk, junk_T)
                            to_HT(hn, hn_T, i)
                        barrier()
                stop_at(0)

                with contextlib.ExitStack() as ps:
                    FB = [sb(f"b_F{i}", [128, L + 32], F32, ps) for i in range(3)]
                    FB_T = TL(3, "F")
                    HB = [sb(f"b_H{i}", [128, L], BF16, ps) for i in range(4)]
                    HB_T = TL(4, "H")
                    wch = Ring(nc, ps, "b_w", [128, 8, 512], BF16, 2)
                    cwa = sb("b_cwa", [128, 2, 3], F32, ps); cwa_T = T()
                    cws = sb("b_cws", [128, 8, 5], F32, ps); cws_T = T()
                    cwc = sb("b_cwc", [128, 2, 34], F32, ps); cwc_T = T()
                    hv = sb("b_hv", [128, 3, 8], F32, ps); hv_T = T()
                    snw = sb("b_snw", [128, 512], F32, ps); snw_T = T()
                    acs = contextlib.ExitStack()
                    ps.callback(acs.close)
                    tmpf = Ring(nc, acs, "b_tmpf", [128, 512], F32, 4)
                    gpb = sb("b_gpb", [128, L + 32], BF16, acs); gpb_T = T()
                    dgw = sb("b_dgw", [128, 31, 128], BF16, acs); dgw_T = T()
                    op("pool", lambda e: e.memset(gpb[:, 0:32], 0.0), w=[gpb_T])
                    dma("sp", cwa[:], cwa_d[:, layer], w=[cwa_T])
                    dma("sp", cws[:], cws_d[:, layer], w=[cws_T])
                    dma("sp", cwc[:], cwc_d[:, layer], w=[cwc_T])
                    dma("sp", hv[:], hv_d[:, layer], w=[hv_T])
                    dma("sp", snw[:], snw_d[:, layer], w=[snw_T])
                    for f in range(3):
                        op("pool", lambda e, f=f: e.memset(FB[f][:, 0:32], 0.0), w=[FB_T[f]])
                    win = win_d[layer].rearrange("(kc p) c -> p kc c", p=128)
                    psrot = [0]

                    def load_w(col0, nch):
                        wt, wt_T = wch.next()
                        dma("pool", wt[:, :, 0:nch * 128], win[:, :, col0:col0 + nch * 128], w=[wt_T])
                        return [(wt[:, :, k * 128:(k + 1) * 128], wt_T) for k in range(nch)]

                    def proj_blk(wt, wt_T, blk):
                        bi = psrot[0]
                        psrot[0] = (bi + 1) % 4
                        p_, p_T = PSF[bi], PSF_T[bi]
                        rT = [wt_T] + HT_T[blk * 4:(blk + 1) * 4]
                        for kc in range(8):
                            op("pe", lambda e, kc=kc: e.matmul(p_[:], lhsT=wt[:, kc, :], rhs=HT[:, kc, blk * 512:(blk + 1) * 512],
                                                               start=(kc == 0), stop=(kc == 7)), r=rT, w=[p_T])
                        return p_, p_T

                    def conv_taps(src, src_T, K, wts, wT, acc, acc_T, bias=None, step=2048):
                        for s0 in range(0, L, step):
                            n = step
                            for j in range(K):
                                o = 32 - (K - 1) + j + s0
                                if j == 0:
                                    if bias is None:
                                        op("dve", lambda e, o=o, s0=s0: e.tensor_scalar(
                                            out=acc[:, s0:s0 + n], in0=src[:, o:o + n], scalar1=wts[:, 0:1], scalar2=None,
                                            op0=ALU.mult), r=[src_T, wT], w=[acc_T])
                                    else:
                                        op("dve", lambda e, o=o, s0=s0: e.tensor_scalar(
                                            out=acc[:, s0:s0 + n], in0=src[:, o:o + n], scalar1=wts[:, 0:1], scalar2=bias,
                                            op0=ALU.mult, op1=ALU.add), r=[src_T, wT], w=[acc_T])
                                else:
                                    op("dve", lambda e, o=o, s0=s0, j=j: e.scalar_tensor_tensor(
                                        out=acc[:, s0:s0 + n], in0=src[:, o:o + n], scalar=wts[:, j:j + 1], in1=acc[:, s0:s0 + n],
                                        op0=ALU.mult, op1=ALU.add), r=[src_T, wT, acc_T], w=[acc_T])

                    for j in range(2):
                        (wb_, wb_T), (wc_, wc_T), (wx_, wx_T) = load_w(j * 384, 3)
                        for blk in range(8):
                            sl = slice(blk * 512, (blk + 1) * 512)
                            slp = slice(32 + blk * 512, 32 + (blk + 1) * 512)
                            pc, pc_T = proj_blk(wc_, wc_T, blk)
                            tf, tf_T = tmpf.next()
                            acopy(tf[:], pc[:], [pc_T], [tf_T])
                            px, px_T = proj_blk(wx_, wx_T, blk)
                            op("dve", lambda e: e.tensor_tensor(out=FB[0][:, slp], in0=tf[:], in1=px[:], op=ALU.mult),
                               r=[tf_T, px_T], w=[FB_T[0]])
                            pb_, pb_T = proj_blk(wb_, wb_T, blk)
                            acopy(FB[1][:, sl], pb_[:], [pb_T], [FB_T[1]])
                        conv_taps(FB[0], FB_T[0], 3, cwa[:, j, :], cwa_T, FB[2], FB_T[2])
                        for s0 in range(0, L, 2048):
                            op("dve", lambda e, s0=s0: e.tensor_tensor(out=HB[j][:, s0:s0 + 2048], in0=FB[2][:, s0:s0 + 2048],
                                                                       in1=FB[1][:, s0:s0 + 2048], op=ALU.mult),
                               r=[FB_T[2], FB_T[1]], w=[HB_T[j]])
                        dma("sp", yT_d[j * 128:(j + 1) * 128, :], HB[j][:], r=[HB_T[j]], w=[yT_T[j]])

                    if layer == 0:
                        stop_at(0.2)
                    for j in range(2):
                        (wa_, wa_T), (wg_, wg_T) = load_w(768 + j * 256, 2)
                        for blk in range(8):
                            slp = slice(32 + blk * 512, 32 + (blk + 1) * 512)
                            pg, pg_T = proj_blk(wg_, wg_T, blk)
                            tf, tf_T = tmpf.next()
                            op("act", lambda e: e.activation(out=tf[:], in_=pg[:], func=AF.Sigmoid), r=[pg_T], w=[tf_T])
                            pa, pa_T = proj_blk(wa_, wa_T, blk)
                            op("dve", lambda e: e.tensor_tensor(out=gpb[:, slp], in0=tf[:], in1=pa[:], op=ALU.mult),
                               r=[tf_T, pa_T], w=[gpb_T])
                        for t_ in range(31):
                            op("dve", lambda e, t_=t_: e.tensor_scalar(out=dgw[:, t_, :], in0=identb[:], scalar1=cwc[:, j, t_:t_ + 1],
                                                                       scalar2=None, op0=ALU.mult), r=[identb_T, cwc_T], w=[dgw_T])
                        for blk in range(8):
                            bi = psrot[0]
                            psrot[0] = (bi + 1) % 4
                            for t_ in range(31):
                                o = 32 - 30 + t_ + blk * 512
                                op("pe", lambda e, t_=t_, o=o: e.matmul(PSF[bi][:], lhsT=dgw[:, t_, :], rhs=gpb[:, o:o + 512],
                                                                        start=(t_ == 0), stop=(t_ == 30)), r=[dgw_T, gpb_T], w=[PSF_T[bi]])
                            op("act", lambda e, blk=blk: e.activation(out=FB[1 + j][:, blk * 512:(blk + 1) * 512], in_=PSF[bi][:],
                                                                      func=AF.Identity, bias=cwc[:, j, 31:32]),
                               r=[PSF_T[bi], cwc_T], w=[FB_T[1 + j]])
                    for blk in range(8):
                        sl = slice(blk * 512, (blk + 1) * 512)
                        sq = []
                        for j in range(2):
                            tf, tf_T = tmpf.next()
                            op("act", lambda e, j=j: e.activation(out=tf[:], in_=FB[1 + j][:, sl], func=AF.Square),
                               r=[FB_T[1 + j]], w=[tf_T])
                            sq.append((tf, tf_T))
                        pm, pm_T = PSF[4], PSF_T[4]
                        pe2, pe2_T = PSF[5], PSF_T[5]
                        for j in range(2):
                            op("pe", lambda e, j=j: e.matmul(pm[:], lhsT=ones256, rhs=FB[1 + j][:, sl], start=(j == 0), stop=(j == 1)),
                               r=[cst_T, FB_T[1 + j]], w=[pm_T])
                        for j in range(2):
                            op("pe", lambda e, j=j: e.matmul(pe2[:], lhsT=ones256, rhs=sq[j][0][:], start=(j == 0), stop=(j == 1)),
                               r=[cst_T, sq[j][1]], w=[pe2_T])
                        mean, mean_T = tmpf.next()
                        acopy(mean[:], pm[:], [pm_T], [mean_T])
                        var, var_T = sq[0]
                        op("dve", lambda e: e.tensor_tensor(out=var[:], in0=mean[:], in1=mean[:], op=ALU.mult), r=[mean_T], w=[var_T])
                        op("dve", lambda e: e.tensor_tensor(out=var[:], in0=pe2[:], in1=var[:], op=ALU.subtract),
                           r=[pe2_T, var_T], w=[var_T])
                        op("dve", lambda e: e.tensor_scalar(out=var[:], in0=var[:], scalar1=EPS, scalar2=None, op0=ALU.add),
                           r=[var_T], w=[var_T])
                        op("act", lambda e: e.activation(out=var[:], in_=var[:], func=AF.Sqrt), r=[var_T], w=[var_T])
                        op("dve", lambda e: e.reciprocal(out=var[:], in_=var[:]), r=[var_T], w=[var_T])
                        t2, t2_T = sq[1]
                        for j in range(2):
                            op("dve", lambda e, j=j: e.tensor_tensor(out=t2[:], in0=FB[1 + j][:, sl], in1=mean[:], op=ALU.subtract),
                               r=[FB_T[1 + j], mean_T], w=[t2_T])
                            op("dve", lambda e: e.tensor_tensor(out=t2[:], in0=t2[:], in1=var[:], op=ALU.mult),
                               r=[t2_T, var_T], w=[t2_T])
                            op("act", lambda e, j=j: e.activation(out=HB[2 + j][:, sl], in_=t2[:], func=AF.Silu,
                                                                  scale=cwc[:, j, 32:33], bias=cwc[:, j, 33:34]),
                               r=[t2_T, cwc_T], w=[HB_T[2 + j]])
                    for j in range(2):
                        dma("sp", yT_d[768 + j * 128:768 + (j + 1) * 128, :], HB[2 + j][:], r=[HB_T[2 + j]], w=[yT_T[6 + j]])

                    if layer == 0:
                        stop_at(0.4)
                    barrier()
                    acs.close()
                    with contextlib.ExitStack() as ss_:
                        rYst = Ring(nc, ss_, "s_yst", [128, 2, 256], BF16, 2)
                        wzz = sb("s_wz", [128, 8, 520], BF16, ss_); wz_T = T()
                        dma("pool", wzz[:], win[:, :, 2304:2824], w=[wz_T])
                        wdt_T = wz_T
                        dtt = sb("s_dt", [128, NT, 8], F32, ss_); dtt_T = T()
                        dtA = sb("s_dtA", [128, NT, 8], F32, ss_); dtA_T = T()
                        acum = sb("s_acum", [128, NT, 8], F32, ss_); acum_T = T()
                        Eall = sb("s_E", [128, NT, 8], F32, ss_); Eall_T = T()
                        cdall = sb("s_cd", [128, NT, 8], F32, ss_); cdall_T = T()
                        dte = sb("s_dte", [128, NT, 8], F32, ss_); dte_T = T()
                        ea = sb("s_ea", [128, 8], F32, ss_); ea_T = T()
                        hs = sb("s_hs", [128, 4, 64], F32, ss_); hs_T = T()
                        hbf = sb("s_hbf", [128, 256], BF16, ss_); hbf_T = T()
                        rG = Ring(nc, ss_, "s_G", [128, 128], F32, 2)
                        rR4 = Ring(nc, ss_, "s_r4", [128, 4, 128], F32, 1)
                        rEx = Ring(nc, ss_, "s_ex", [128, 4, 128], F32, 1)
                        rM = Ring(nc, ss_, "s_M", [128, 4, 128], BF16, 2)
                        rXs = Ring(nc, ss_, "s_xs", [128, 4, 64], BF16, 2)
                        rXd = Ring(nc, ss_, "s_xd", [128, 4, 64], BF16, 2)
                        rXd2 = Ring(nc, ss_, "s_xd2", [128, 4, 64], BF16, 2)
                        rBt = Ring(nc, ss_, "s_bt", [128, 128], BF16, 2)
                        rT1 = Ring(nc, ss_, "s_t1", [128, 4, 64], F32, 2)
                        rT3 = Ring(nc, ss_, "s_t3", [128, 4, 64], F32, 1)
                        rSz = Ring(nc, ss_, "s_sz", [128, 256], F32, 3)
                        rYo = Ring(nc, ss_, "s_yo", [128, 256], BF16, 2)
                        rSm = Ring(nc, ss_, "s_sm", [128, 4], F32, 3)
                        junk = sb("s_junk", [128, 256], BF16, ss_); junk_T = T()

                        pdt, pdt_T = PSF[4], PSF_T[4]
                        for i in range(NT):
                            for kc in range(8):
                                op("pe", lambda e, kc=kc, i=i: e.matmul(pdt[:, i * 8:(i + 1) * 8], lhsT=HT[:, kc, i * 128:(i + 1) * 128],
                                                                        rhs=wzz[:, kc, 512:520], start=(kc == 0), stop=(kc == 7)),
                                   r=[HT_T[i], wdt_T], w=[pdt_T])
                        pdt3 = pdt[:, 0:256].rearrange("p (c h) -> p c h", h=8)
                        op("dve", lambda e: e.tensor_tensor(out=dtt[:], in0=pdt3, in1=bc(hv[:, 0:1, :], [128, NT, 8]), op=ALU.add),
                           r=[pdt_T, hv_T], w=[dtt_T])
                        op("act", lambda e: e.activation(out=dtt[:], in_=dtt[:], func=AF.Exp), r=[dtt_T], w=[dtt_T])
                        op("dve", lambda e: e.tensor_scalar(out=dtt[:], in0=dtt[:], scalar1=1.0, scalar2=None, op0=ALU.add),
                           r=[dtt_T], w=[dtt_T])
                        op("act", lambda e: e.activation(out=dtt[:], in_=dtt[:], func=AF.Ln), r=[dtt_T], w=[dtt_T])
                        if layer == 0:
                            stop_at(0.5)
                        op("act", lambda e: e.activation(out=ea[:], in_=hv[:, 1, :], func=AF.Exp), r=[hv_T], w=[ea_T])
                        op("dve", lambda e: e.scalar_tensor_tensor(out=dtA[:], in0=dtt[:], scalar=-1.0,
                                                                   in1=bc(ea[:].rearrange("p (o h) -> p o h", o=1), [128, NT, 8]),
                                                                   op0=ALU.mult, op1=ALU.mult), r=[dtt_T, ea_T], w=[dtA_T])
                        fl = lambda t_: t_[:].rearrange("p c h -> p (c h)")
                        pac, pac_T = PSF[5], PSF_T[5]
                        op("pe", lambda e: e.matmul(pac[:, 0:256], lhsT=LEm, rhs=fl(dtA), start=True, stop=True),
                           r=[cst_T, dtA_T], w=[pac_T])
                        op("pe", lambda e: e.matmul(pac[:, 256:512], lhsT=ones, rhs=fl(dtA), start=True, stop=True),
                           r=[cst_T, dtA_T], w=[pac_T])
                        acopy(fl(acum), pac[:, 0:256], [pac_T], [acum_T])
                        op("act", lambda e: e.activation(out=fl(Eall), in_=pac[:, 0:256], func=AF.Exp), r=[pac_T], w=[Eall_T])
                        op("act", lambda e: e.activation(out=fl(cdall), in_=pac[:, 256:512], func=AF.Exp), r=[pac_T], w=[cdall_T])
                        op("dve", lambda e: e.tensor_tensor(out=fl(dte), in0=pac[:, 256:512], in1=fl(acum), op=ALU.subtract),
                           r=[pac_T, acum_T], w=[dte_T])
                        op("act", lambda e: e.activation(out=fl(dte), in_=fl(dte), func=AF.Exp), r=[dte_T], w=[dte_T])

                        if layer == 0:
                            stop_at(0.6)

                        def hb(t_, c, g):
                            return t_[:, c:c + 1, g * 4:(g + 1) * 4].rearrange("p o h -> p h o")

                        for g in range(2):
                            cb = 1280 + g * 512
                            wqs = load_w(cb, 4)
                            for q in range(4):
                                wq, wq_T = wqs[q]
                                fpad, fpad_T = FB[q % 2], FB_T[q % 2]
                                op("pool", lambda e: e.memset(fpad[:, 0:32], 0.0), w=[fpad_T])
                                for blk in range(8):
                                    pq, pq_T = proj_blk(wq, wq_T, blk)
                                    acopy(fpad[:, 32 + blk * 512:32 + (blk + 1) * 512], pq[:], [pq_T], [fpad_T])
                                ci = g * 4 + q
                                conv_taps(fpad, fpad_T, 4, cws[:, ci, 0:4], cws_T, FB[2], FB_T[2], bias=cws[:, ci, 4:5])
                                for s0 in range(0, L, 2048):
                                    op("act", lambda e, s0=s0, q=q: e.activation(out=HB[q][:, s0:s0 + 2048], in_=FB[2][:, s0:s0 + 2048],
                                                                                func=AF.Silu), r=[FB_T[2]], w=[HB_T[q]])
                            if layer == 0 and g == 0:
                                stop_at(0.7)
                            op("dve", lambda e: e.memset(hs[:], 0.0), w=[hs_T])
                            op("dve", lambda e: e.memset(hbf[:], 0.0), w=[hbf_T])
                            def front(c):
                                sl = slice(c * 128, (c + 1) * 128)
                                op("pe", lambda e: e.matmul(PSF[1][:, 0:128], lhsT=HB[2][:, sl], rhs=HB[3][:, sl], start=True, stop=True),
                                   r=[HB_T[2], HB_T[3]], w=[PSF_T[1]])
                                G, G_T = rG.next()
                                op("dve", lambda e: e.tensor_tensor(out=G[:], in0=PSF[1][:, 0:128], in1=LEm, op=ALU.mult),
                                   r=[PSF_T[1], cst_T], w=[G_T])
                                r4, r4_T = rR4.next()
                                op("dve", lambda e: e.tensor_tensor(out=r4[:], in0=bc(LEm.rearrange("p (o l) -> p o l", o=1), [128, 4, 128]),
                                                                    in1=bc(hb(dtA, c, g), [128, 4, 128]), op=ALU.mult),
                                   r=[cst_T, dtA_T], w=[r4_T])
                                sgi = 0 if c % 2 == 0 else 5
                                op("pe", lambda e: e.matmul(PSF[sgi][:], lhsT=LTs, rhs=r4[:].rearrange("p h l -> p (h l)"),
                                                            start=True, stop=True), r=[cst_T, r4_T], w=[PSF_T[sgi]])
                                ex, ex_T = rEx.next()
                                op("act", lambda e: e.activation(out=ex[:].rearrange("p h l -> p (h l)"), in_=PSF[sgi][:], func=AF.Exp),
                                   r=[PSF_T[sgi]], w=[ex_T])
                                M, M_T = rM.next()
                                op("pool", lambda e: e.tensor_tensor(out=M[:], in0=ex[:],
                                                                     in1=bc(G[:].rearrange("p (o l) -> p o l", o=1), [128, 4, 128]),
                                                                     op=ALU.mult), r=[ex_T, G_T], w=[M_T])
                                for q in range(2):
                                    op("pe", lambda e, q=q: e.transpose(out=PSB[0][:, q * 128:(q + 1) * 128], in_=HB[q][:, sl],
                                                                        identity=identb[:]), r=[HB_T[q], identb_T], w=[PSB_T[0]])
                                xs, xs_T = rXs.next()
                                px3 = PSB[0][:, 0:256].rearrange("p (h d) -> p h d", h=4)
                                acopy(xs[:], px3, [PSB_T[0]], [xs_T])
                                xd, xd_T = rXd.next()
                                op("dve", lambda e: e.tensor_tensor(out=xd[:], in0=px3, in1=bc(hb(dtt, c, g), [128, 4, 64]), op=ALU.mult),
                                   r=[PSB_T[0], dtt_T], w=[xd_T])
                                xd2, xd2_T = rXd2.next()
                                op("pool", lambda e: e.tensor_tensor(out=xd2[:], in0=xd[:], in1=bc(hb(dte, c, g), [128, 4, 64]), op=ALU.mult),
                                   r=[xd_T, dte_T], w=[xd2_T])
                                op("pe", lambda e: e.transpose(out=PSB[0][:, 256:384], in_=HB[2][:, sl], identity=identb[:]),
                                   r=[HB_T[2], identb_T], w=[PSB_T[0]])
                                bt, bt_T = rBt.next()
                                acopy(bt[:], PSB[0][:, 256:384], [PSB_T[0]], [bt_T])
                                for kc in range(8):
                                    op("pe", lambda e, kc=kc: e.matmul(PSF[4][:, 0:256], lhsT=HT[:, kc, sl], rhs=wzz[:, kc, g * 256:(g + 1) * 256],
                                                                       start=(kc == 0), stop=(kc == 7)), r=[HT_T[c], wz_T], w=[PSF_T[4]])
                                sz, sz_T = rSz.next()
                                op("act", lambda e: e.activation(out=sz[:], in_=PSF[4][:, 0:256], func=AF.Exp, scale=-1.0),
                                   r=[PSF_T[4]], w=[sz_T])
                                op("dve", lambda e: e.tensor_scalar(out=sz[:], in0=sz[:], scalar1=1.0, scalar2=None, op0=ALU.add),
                                   r=[sz_T], w=[sz_T])
                                op("dve", lambda e: e.reciprocal(out=sz[:], in_=sz[:]), r=[sz_T], w=[sz_T])
                                op("dve", lambda e: e.tensor_tensor(out=sz[:], in0=sz[:], in1=PSF[4][:, 0:256], op=ALU.mult),
                                   r=[sz_T, PSF_T[4]], w=[sz_T])
                                return (M, M_T, xs, xs_T, xd, xd_T, xd2, xd2_T, bt, bt_T, sz, sz_T)

                            def back(c, P):
                                M, M_T, xs, xs_T, xd, xd_T, xd2, xd2_T, bt, bt_T, sz, sz_T = P
                                sl = slice(c * 128, (c + 1) * 128)
                                for r_ in range(4):
                                    op("pe", lambda e, r_=r_: e.matmul(PSF[2][:, r_ * 64:(r_ + 1) * 64], lhsT=M[:, r_, :], rhs=xd[:, r_, :],
                                                                       start=True, stop=True), r=[M_T, xd_T], w=[PSF_T[2]])
                                op("pe", lambda e: e.matmul(PSF[3][:, 0:256], lhsT=HB[3][:, sl], rhs=hbf[:], start=True, stop=True),
                                   r=[HB_T[3], hbf_T], w=[PSF_T[3]])
                                op("pe", lambda e: e.matmul(PSF[3][:, 256:512], lhsT=bt[:], rhs=xd2[:].rearrange("p h d -> p (h d)"),
                                                            start=True, stop=True), r=[bt_T, xd2_T], w=[PSF_T[3]])
                                t1, t1_T = rT1.next()
                                op("dve", lambda e: e.tensor_tensor(out=t1[:], in0=PSF[3][:, 0:256].rearrange("p (h d) -> p h d", h=4),
                                                                    in1=bc(hb(Eall, c, g), [128, 4, 64]), op=ALU.mult),
                                   r=[PSF_T[3], Eall_T], w=[t1_T])
                                op("dve", lambda e: e.tensor_tensor(out=t1[:], in0=t1[:],
                                                                    in1=PSF[2][:, 0:256].rearrange("p (h d) -> p h d", h=4), op=ALU.add),
                                   r=[t1_T, PSF_T[2]], w=[t1_T])
                                t3, t3_T = rT3.next()
                                op("pool", lambda e: e.tensor_tensor(out=t3[:], in0=xs[:],
                                                                     in1=bc(hv[:, 2:3, g * 4:(g + 1) * 4].rearrange("p o h -> p h o"), [128, 4, 64]),
                                                                     op=ALU.mult), r=[xs_T, hv_T], w=[t3_T])
                                op("dve", lambda e: e.tensor_tensor(out=t1[:], in0=t1[:], in1=t3[:], op=ALU.add), r=[t1_T, t3_T], w=[t1_T])
                                op("dve", lambda e: e.tensor_tensor(out=hs[:], in0=hs[:], in1=bc(hb(cdall, c, g), [128, 4, 64]), op=ALU.mult),
                                   r=[hs_T, cdall_T], w=[hs_T])
                                op("dve", lambda e: e.tensor_tensor(out=hs[:], in0=hs[:],
                                                                    in1=PSF[3][:, 256:512].rearrange("p (h d) -> p h d", h=4), op=ALU.add),
                                   r=[hs_T, PSF_T[3]], w=[hs_T])
                                acopy(hbf[:], hs[:].rearrange("p h d -> p (h d)"), [hs_T], [hbf_T])
                                return (t1, t1_T, sz, sz_T)

                            def back2(c, Q):
                                t1, t1_T, sz, sz_T = Q
                                sl = slice(c * 128, (c + 1) * 128)
                                op("dve", lambda e: e.tensor_tensor(out=sz[:], in0=sz[:], in1=t1[:].rearrange("p h d -> p (h d)"), op=ALU.mult),
                                   r=[sz_T, t1_T], w=[sz_T])
                                sm, sm_T = rSm.next()
                                op("act", lambda e: e.activation(out=junk[:], in_=sz[:], func=AF.Square, accum_out=sm[:, 0:1]),
                                   r=[sz_T], w=[junk_T, sm_T])
                                rstd_from_ss(sm[:, 0:1], 256, sm[:, 1:2], sm[:, 2:3], sm_T)
                                yo, yo_T = rYo.next()
                                op("dve", lambda e: e.scalar_tensor_tensor(out=yo[:], in0=sz[:], scalar=sm[:, 2:3],
                                                                           in1=snw[:, g * 256:(g + 1) * 256], op0=ALU.mult, op1=ALU.mult),
                                   r=[sz_T, sm_T, snw_T], w=[yo_T])
                                for q in range(2):
                                    op("pe", lambda e, q=q: e.transpose(out=PSB[1][:, q * 128:(q + 1) * 128], in_=yo[:, q * 128:(q + 1) * 128],
                                                                        identity=identb[:]), r=[yo_T, identb_T], w=[PSB_T[1]])
                                if c % 2 == 0:
                                    ystate["y"] = rYst.next()
                                yst, yst_T = ystate["y"]
                                acopy(yst[:, :, (c % 2) * 128:(c % 2 + 1) * 128], PSB[1][:, 0:256].rearrange("p (q t) -> p q t", q=2),
                                      [PSB_T[1]], [yst_T])
                                if c % 2 == 1:
                                    for q in range(2):
                                        r0 = 256 + g * 256 + q * 128
                                        dma("sp", yT_d[r0:r0 + 128, (c - 1) * 128:(c + 1) * 128], yst[:, q, :], r=[yst_T],
                                            w=[yT_T[2 + g * 2 + q]] if c == NT - 1 else [T()])

                            ystate = {}
                            P_ = front(0)
                            Qp = None
                            for c in range(NT):
                                Pn = front(c + 1) if c + 1 < NT else None
                                Q_ = back(c, P_)
                                if Qp is not None:
                                    back2(c - 1, Qp)
                                Qp = Q_
                                P_ = Pn
                            back2(NT - 1, Qp)
                        barrier()
                    barrier()
                stop_at(1 + 10 * layer)

                moe = (layer == 1)
                if moe:
                    hstack.close()
                with contextlib.ExitStack() as ps:
                    wo = sb("c_wo", [128, 8, D], BF16, ps); wo_T = T()
                    dma("pool", wo[:], wout_d[layer].rearrange("(cc p) d -> p cc d", p=128), w=[wo_T])
                    ytl = Ring(nc, ps, "c_y", [128, 8, 512], BF16, 2)
                    xin = Ring(nc, ps, "c_x", [128, D], F32, 3)
                    smr = Ring(nc, ps, "c_sm", [128, 4], F32, 3)
                    junk = sb("c_junk", [128, D], BF16, ps); junk_T = T()
                    dma("sp", nwb[:], nw_d[:, 1 + 2 * layer, :], w=[nwb_T])
                    yT_v = yT_d.rearrange("(cc p) t -> p cc t", p=128)
                    if not moe:
                        hnr = Ring(nc, ps, "c_hn", [128, D], BF16, 2)
                    else:
                        hn32 = Ring(nc, ps, "c_h32", [128, D], F32, 2)
                        h3T = Ring(nc, ps, "c_h3T", [128, 8, 128], F32, 2)
                        H3 = sb("m_H3", [128, NT, D], BF16, ps); H3_T = TL(NT, "H3")
                        wr = sb("m_wr", [128, 8, NE], F32, ps); wr_T = T()
                        dma("sp", wr[:], wr_d.rearrange("(kc p) e -> p kc e", p=128), w=[wr_T])
                        lgr = Ring(nc, ps, "m_lg", [128, 24], F32, 3)
                        M1m = sb("m_M1", [128, NT, NE], F32, ps)
                        M2m = sb("m_M2", [128, NT, NE], F32, ps)
                        RK = sb("m_RK", [128, NT, NE], F32, ps)
                        tot = sb("m_tot", [128, NE], F32, ps); tot_T = T()
                        op("dve", lambda e: e.memset(tot[:], 0.0), w=[tot_T])
                    for blk in range(8):
                        yl, yl_T = ytl.next()
                        dma("sp", yl[:], yT_v[:, :, blk * 512:(blk + 1) * 512], r=yT_T, w=[yl_T])
                        for s in range(4):
                            i = blk * 4 + s
                            xt, xt_T = xin.next()
                            dma("sp", xt[:], res_src[i * 128:(i + 1) * 128, :], r=([xres_T[i]] if layer else []), w=[xt_T])
                            for dh in range(2):
                                bi = (2 * i + dh) % 4
                                for cc in range(8):
                                    op("pe", lambda e, cc=cc: e.matmul(PSF[bi][:], lhsT=yl[:, cc, s * 128:(s + 1) * 128],
                                                                       rhs=wo[:, cc, dh * 512:(dh + 1) * 512],
                                                                       start=(cc == 0), stop=(cc == 7)), r=[yl_T, wo_T], w=[PSF_T[bi]])
                                op("dve", lambda e: e.tensor_tensor(out=xt[:, dh * 512:(dh + 1) * 512], in0=xt[:, dh * 512:(dh + 1) * 512],
                                                                    in1=PSF[bi][:], op=ALU.add), r=[xt_T, PSF_T[bi]], w=[xt_T])
                            dma("sp", xres_d[i * 128:(i + 1) * 128, :], xt[:], r=[xt_T], w=[xres_T[i]])
                            if not moe:
                                hn, hn_T = hnr.next()
                                norm_tile(xt[:], xt_T, hn[:], hn_T, smr, junk, junk_T)
                                to_HT(hn, hn_T, i)
                            else:
                                h32, h32_T = hn32.next()
                                norm_tile(xt[:], xt_T, h32[:], h32_T, smr, junk, junk_T)
                                acopy(H3[:, i, :], h32[:], [h32_T], [H3_T[i]], eng="pool")
                                for k in range(8):
                                    pf = PSF[4 + k // 4]
                                    op("pe", lambda e, k=k: e.transpose(out=pf[:, (k % 4) * 128:(k % 4 + 1) * 128],
                                                                        in_=h32[:, k * 128:(k + 1) * 128], identity=ident),
                                       r=[h32_T, cst_T], w=[PSF_T[4 + k // 4]])
                                hT3, hT3_T = h3T.next()
                                acopy(hT3[:, 0:4, :], PSF[4][:].rearrange("p (k t) -> p k t", k=4), [PSF_T[4]], [hT3_T])
                                acopy(hT3[:, 4:8, :], PSF[5][:].rearrange("p (k t) -> p k t", k=4), [PSF_T[5]], [hT3_T], eng="dve")
                                pl, pl_T = PSB[0], PSB_T[0]
                                plf = PSF[(2 * i + 2) % 4]
                                plf_T = PSF_T[(2 * i + 2) % 4]
                                for kc in range(8):
                                    op("pe", lambda e, kc=kc: e.matmul(plf[:, 0:NE], lhsT=hT3[:, kc, :], rhs=wr[:, kc, :],
                                                                       start=(kc == 0), stop=(kc == 7)), r=[hT3_T, wr_T], w=[plf_T])
                                lg, lg_T = lgr.next()
                                acopy(lg[:, 0:8], plf[:, 0:NE], [plf_T], [lg_T])
                                dv = lambda fn, r=(), w=(): op("dve", fn, r=list(r), w=list(w))
                                dv(lambda e: e.max(out=lg[:, 8:16], in_=lg[:, 0:8]), [lg_T], [lg_T])
                                dv(lambda e: e.tensor_scalar(out=M1m[:, i, :], in0=lg[:, 0:8], scalar1=lg[:, 8:9], scalar2=None,
                                                             op0=ALU.is_ge), [lg_T], [rt_T])
                                dv(lambda e: e.tensor_scalar(out=lg[:, 16:24], in0=lg[:, 0:8], scalar1=lg[:, 9:10], scalar2=None,
                                                             op0=ALU.is_ge), [lg_T], [lg_T])
                                dv(lambda e: e.tensor_tensor(out=M2m[:, i, :], in0=lg[:, 16:24], in1=M1m[:, i, :], op=ALU.subtract),
                                   [lg_T, rt_T], [rt_T])
                                dv(lambda e: e.tensor_tensor(out=lg[:, 10:11], in0=lg[:, 9:10], in1=lg[:, 8:9], op=ALU.subtract),
                                   [lg_T], [lg_T])
                                op("act", lambda e: e.activation(out=lg[:, 10:11], in_=lg[:, 10:11], func=AF.Exp), r=[lg_T], w=[lg_T])
                                dv(lambda e: e.tensor_scalar(out=lg[:, 11:12], in0=lg[:, 10:11], scalar1=1.0, scalar2=None, op0=ALU.add),
                                   [lg_T], [lg_T])
                                dv(lambda e: e.reciprocal(out=GT[:, i, 0:1], in_=lg[:, 11:12]), [lg_T], [rt_T])
                                dv(lambda e: e.tensor_tensor(out=GT[:, i, 1:2], in0=lg[:, 10:11], in1=GT[:, i, 0:1], op=ALU.mult),
                                   [lg_T, rt_T], [rt_T])
                                prk = PSF[(2 * i + 3) % 4]
                                prk_T = PSF_T[(2 * i + 3) % 4]
                                op("pe", lambda e: e.matmul(prk[:, 0:8], lhsT=SUm, rhs=lg[:, 16:24], start=True, stop=True),
                                   r=[cst_T, lg_T], w=[prk_T])
                                op("pe", lambda e: e.matmul(prk[:, 8:16], lhsT=ones, rhs=lg[:, 16:24], start=True, stop=True),
                                   r=[cst_T, lg_T], w=[prk_T])
                                dv(lambda e: e.tensor_tensor(out=RK[:, i, :], in0=prk[:, 0:8], in1=tot[:], op=ALU.add),
                                   [prk_T, tot_T], [rt_T])
                                dv(lambda e: e.tensor_tensor(out=tot[:], in0=tot[:], in1=prk[:, 8:16], op=ALU.add),
                                   [prk_T, tot_T], [tot_T])
                    if moe:
                        zt = sb("m_zero", [128, 8, D], BF16, ps); zt_T = T()
                        op("pool", lambda e: e.memset(zt[:], 0.0), w=[zt_T])
                        hmz_T = T("hmz")
                        ybz_T = T("ybz")
                        hm_v = hm_d.rearrange("(b p) d -> p b d", p=128)
                        for b0 in range(0, NBLK + 1, 8):
                            nb = min(8, NBLK + 1 - b0)
                            dma("sp", hm_v[:, b0:b0 + nb, :], zt[:, 0:nb, :], r=[zt_T], w=[hmz_T] if b0 == 0 else [T()])
                        hmz_all = hmz_T
                        zf = sb("m_zf", [128, D], F32, ps); zf_T = T()
                        op("pool", lambda e: e.memset(zf[:], 0.0), w=[zf_T])
                        dma("sp", yb_d[NROW:NROW + 128, :], zf[:], r=[zf_T], w=[ybz_T])
                        RS = sb("m_RS", [128, NT, NE], F32, ps)
                        prod = sb("m_prod", [128, NT, NE], F32, ps)
                        dstf = sb("m_dstf", [128, 2, NT], F32, ps)
                        rkf = sb("m_rkf", [128, NT], F32, ps)
                        stv = sb("m_stv", [128, NE], F32, ps)
                        for e_ in range(NE):
                            dv(lambda e, e_=e_: e.memset(stv[:, e_:e_ + 1], float(e_ * CAPROWS)), [], [rt_T])
                        dv(lambda e: e.tensor_tensor(out=RS[:], in0=RK[:], in1=bc(stv[:].rearrange("p (o e) -> p o e", o=1), [128, NT, NE]),
                                                     op=ALU.add), [rt_T], [rt_T])
                        for k, Mk in enumerate((M1m, M2m)):
                            dv(lambda e, Mk=Mk: e.tensor_tensor(out=prod[:], in0=RS[:], in1=Mk[:], op=ALU.mult), [rt_T], [rt_T])
                            dv(lambda e, k=k: e.tensor_reduce(out=dstf[:, k, :], in_=prod[:], axis=AX.X, op=ALU.add), [rt_T], [rt_T])
                            dv(lambda e, Mk=Mk: e.tensor_tensor(out=prod[:], in0=RK[:], in1=Mk[:], op=ALU.mult), [rt_T], [rt_T])
                            dv(lambda e: e.tensor_reduce(out=rkf[:], in_=prod[:], axis=AX.X, op=ALU.add), [rt_T], [rt_T])
                            dv(lambda e: e.tensor_scalar(out=rkf[:], in0=rkf[:], scalar1=float(CAPROWS), scalar2=1.0e6,
                                                         op0=ALU.is_ge, op1=ALU.mult), [rt_T], [rt_T])
                            dv(lambda e, k=k: e.tensor_tensor(out=dstf[:, k, :], in0=dstf[:, k, :], in1=rkf[:], op=ALU.add), [rt_T], [rt_T])
                            dv(lambda e, k=k: e.tensor_scalar(out=dstf[:, k, :], in0=dstf[:, k, :], scalar1=float(NROW), scalar2=None,
                                                              op0=ALU.min), [rt_T], [rt_T])
                        dv(lambda e: e.tensor_copy(out=desti[:], in_=dstf[:]), [rt_T], [rt_T])
                        hms_T = TL(2 * NT, "hms")
                        barrier()
                        for i in range(NT):
                            for k in range(2):
                                dma("pool", hm_d[:, :], H3[:, i, :], r=[H3_T[i], rt_T], w=[hms_T[2 * i + k]],
                                    indirect=dict(out_offset=bass.IndirectOffsetOnAxis(ap=desti[:, k, i:i + 1], axis=0), in_offset=None))
                    barrier()
                stop_at(2 + 10 * layer)

                if not moe:
                    with contextlib.ExitStack() as ps:
                        wd = sb("d_wd", [128, NFF, D], BF16, ps); wd_T = T()
                        dma("pool", wd[:], ffd_d.rearrange("(j p) d -> p j d", p=128), w=[wd_T])
                        aT = sb("d_aT", [128, NFF, 512], BF16, ps); aT_T = TL(NFF, "aT")
                        wgu = Ring(nc, ps, "d_wgu", [128, 2, 8, 256], BF16, 3)
                        sgr = Ring(nc, ps, "d_sg", [128, 512], F32, 2)
                        xin = Ring(nc, ps, "d_x", [128, D], F32, 3)
                        hnr = Ring(nc, ps, "d_hn", [128, D], BF16, 2)
                        smr = Ring(nc, ps, "d_sm", [128, 4], F32, 3)
                        junk = sb("d_junk", [128, D], BF16, ps); junk_T = T()
                        dma("sp", nwb[:], nw_d[:, 2, :], w=[nwb_T])
                        ffg_v = ffg_d.rearrange("(kc p) f -> p kc f", p=128)
                        ffu_v = ffu_d.rearrange("(kc p) f -> p kc f", p=128)
                        for tb in range(8):
                            tsl = slice(tb * 512, (tb + 1) * 512)
                            for j in range(NFF):
                                if j % 2 == 0:
                                    wt2, wt_T = wgu.next()
                                    dma("pool", wt2[:, 0], ffg_v[:, :, j * 128:(j + 2) * 128], w=[wt_T])
                                    dma("pool", wt2[:, 1], ffu_v[:, :, j * 128:(j + 2) * 128], w=[wt_T])
                                wt = wt2[:, :, :, (j % 2) * 128:(j % 2 + 1) * 128]
                                pg, pg_T = PSF[j % 2], PSF_T[j % 2]
                                pu, pu_T = PSF[2 + j % 2], PSF_T[2 + j % 2]
                                rT = [wt_T] + HT_T[tb * 4:(tb + 1) * 4]
                                for kc in range(8):
                                    op("pe", lambda e, kc=kc: e.matmul(pg[:], lhsT=wt[:, 0, kc, :], rhs=HT[:, kc, tsl],
                                                                       start=(kc == 0), stop=(kc == 7)), r=rT, w=[pg_T])
                                for kc in range(8):
                                    op("pe", lambda e, kc=kc: e.matmul(pu[:], lhsT=wt[:, 1, kc, :], rhs=HT[:, kc, tsl],
                                                                       start=(kc == 0), stop=(kc == 7)), r=rT, w=[pu_T])
                                sg, sg_T = sgr.next()
                                op("act", lambda e: e.activation(out=sg[:], in_=pg[:], func=AF.Silu), r=[pg_T], w=[sg_T])
                                op("dve", lambda e, j=j: e.tensor_tensor(out=aT[:, j, :], in0=sg[:], in1=pu[:], op=ALU.mult),
                                   r=[sg_T, pu_T], w=[aT_T[j]])
                            for s in range(4):
                                i = tb * 4 + s
                                xt, xt_T = xin.next()
                                dma("sp", xt[:], xres_d[i * 128:(i + 1) * 128, :], r=[xres_T[i]], w=[xt_T])
                                for dh in range(2):
                                    pd, pd_T = PSF[4 + dh], PSF_T[4 + dh]
                                    for j in range(NFF):
                                        op("pe", lambda e, j=j: e.matmul(pd[:], lhsT=aT[:, j, s * 128:(s + 1) * 128],
                                                                         rhs=wd[:, j, dh * 512:(dh + 1) * 512],
                                                                         start=(j == 0), stop=(j == NFF - 1)), r=[aT_T[j], wd_T], w=[pd_T])
                                    op("dve", lambda e: e.tensor_tensor(out=xt[:, dh * 512:(dh + 1) * 512], in0=xt[:, dh * 512:(dh + 1) * 512],
                                                                        in1=pd[:], op=ALU.add), r=[xt_T, pd_T], w=[xt_T])
                                dma("sp", xres_d[i * 128:(i + 1) * 128, :], xt[:], r=[xt_T], w=[xres_T[i]])
                                hn, hn_T = hnr.next()
                                norm_tile(xt[:], xt_T, hn[:], hn_T, smr, junk, junk_T)
                                to_HT(hn, hn_T, i)
                        barrier()
                else:
                    with contextlib.ExitStack() as ps:
                        XT = sb("e_XT", [128, CAP, 8, 128], BF16, ps); XT_T = TL(CAP, "XT")
                        aT = sb("e_aT", [128, NFE, CAP * 128], BF16, ps); aT_T = TL(NFE, "eaT")
                        xb = Ring(nc, ps, "e_xb", [128, D], BF16, 3)
                        wgu = Ring(nc, ps, "e_wgu", [128, 2, 8, 256], BF16, 4)
                        wdq = Ring(nc, ps, "e_wd", [128, NFE, 256], BF16, 2)
                        sgr = Ring(nc, ps, "e_sg", [128, 512], F32, 2)
                        yor = Ring(nc, ps, "e_yo", [128, 512], F32, 3)
                        yb_T = TL(NBLK, "yb")
                        for ex_ in range(NE):
                            for bb in range(CAP):
                                b = ex_ * CAP + bb
                                xt, xt_T = xb.next()
                                dma("sp", xt[:], hm_d[b * 128:(b + 1) * 128, :], r=[hmz_all] + hms_T, w=[xt_T])
                                pb, pb_T = PSB[bb % 2], PSB_T[bb % 2]
                                for k in range(8):
                                    op("pe", lambda e, k=k: e.transpose(out=pb[:, k * 128:(k + 1) * 128], in_=xt[:, k * 128:(k + 1) * 128],
                                                                        identity=identb[:]), r=[xt_T, identb_T], w=[pb_T])
                                acopy(XT[:, bb, :, :], pb[:].rearrange("p (k t) -> p k t", k=8), [pb_T], [XT_T[bb]],
                                      eng=("act" if bb % 2 == 0 else "dve"))
                            mg_v = mg_d[ex_].rearrange("(kc p) f -> p kc f", p=128)
                            mu_v = mu_d[ex_].rearrange("(kc p) f -> p kc f", p=128)
                            md_v = md_d[ex_].rearrange("(j p) d -> p j d", p=128)
                            for j in range(NFE):
                                if j % 2 == 0:
                                    wt2, wt_T = wgu.next()
                                    dma("pool", wt2[:, 0], mg_v[:, :, j * 128:(j + 2) * 128], w=[wt_T])
                                    dma("pool", wt2[:, 1], mu_v[:, :, j * 128:(j + 2) * 128], w=[wt_T])
                                wt = wt2[:, :, :, (j % 2) * 128:(j % 2 + 1) * 128]
                                for qd in range(CAP // 4):
                                    pg, pg_T = PSF[qd % 2], PSF_T[qd % 2]
                                    pu, pu_T = PSF[2 + qd % 2], PSF_T[2 + qd % 2]
                                    rT = [wt_T] + XT_T[qd * 4:(qd + 1) * 4]
                                    for gu, pp in ((0, pg), (1, pu)):
                                        for kc in range(8):
                                            op("pe", lambda e, kc=kc, gu=gu, pp=pp: e.matmul(
                                                pp[:].rearrange("p (b t) -> p b t", b=4), lhsT=wt[:, gu, kc, :],
                                                rhs=XT[:, qd * 4:(qd + 1) * 4, kc, :], start=(kc == 0), stop=(kc == 7)),
                                               r=rT, w=[pg_T if gu == 0 else pu_T])
                                    sg, sg_T = sgr.next()
                                    op("act", lambda e: e.activation(out=sg[:], in_=pg[:], func=AF.Silu), r=[pg_T], w=[sg_T])
                                    op("dve", lambda e, j=j, qd=qd: e.tensor_tensor(out=aT[:, j, qd * 512:(qd + 1) * 512], in0=sg[:],
                                                                                     in1=pu[:], op=ALU.mult),
                                       r=[sg_T, pu_T], w=[aT_T[j]])
                            for dq in range(4):
                                wq, wq_T = wdq.next()
                                dma("pool", wq[:], md_v[:, :, dq * 256:(dq + 1) * 256], w=[wq_T])
                                for b2 in range(CAP // 2):
                                    pd, pd_T = PSF[4 + b2 % 2], PSF_T[4 + b2 % 2]
                                    for u in range(2):
                                        bb = b2 * 2 + u
                                        for j in range(NFE):
                                            op("pe", lambda e, j=j, u=u, bb=bb: e.matmul(
                                                pd[:, u * 256:(u + 1) * 256], lhsT=aT[:, j, bb * 128:(bb + 1) * 128], rhs=wq[:, j, :],
                                                start=(j == 0), stop=(j == NFE - 1)), r=[aT_T[j], wq_T], w=[pd_T])
                                    yo, yo_T = yor.next()
                                    acopy(yo[:], pd[:], [pd_T], [yo_T], eng=("act" if b2 % 2 == 0 else "dve"))
                                    for u in range(2):
                                        b = ex_ * CAP + b2 * 2 + u
                                        dma("sp", yb_d[b * 128:(b + 1) * 128, dq * 256:(dq + 1) * 256], yo[:, u * 256:(u + 1) * 256],
                                            r=[yo_T], w=[yb_T[b]] if dq == 3 else [T()])
                        barrier()
                    with contextlib.ExitStack() as ps:
                        xin = Ring(nc, ps, "f_x", [128, D], F32, 3)
                        y0r = Ring(nc, ps, "f_y0", [128, D], F32, 2)
                        y1r = Ring(nc, ps, "f_y1", [128, D], F32, 2)
                        outr = Ring(nc, ps, "f_o", [128, D], F32, 2)
                        smr = Ring(nc, ps, "f_sm", [128, 4], F32, 3)
                        junk = sb("f_junk", [128, D], BF16, ps); junk_T = T()
                        dma("sp", nwb[:], nw_d[:, 4, :], w=[nwb_T])
                        for i in range(NT):
                            xt, xt_T = xin.next()
                            dma("sp", xt[:], xres_d[i * 128:(i + 1) * 128, :], r=[xres_T[i]], w=[xt_T])
                            ys = []
                            for k, rr in enumerate((y0r, y1r)):
                                yk, yk_T = rr.next()
                                dma("pool", yk[:], yb_d[:, :], r=yb_T + [ybz_T, rt_T], w=[yk_T],
                                    indirect=dict(out_offset=None, in_offset=bass.IndirectOffsetOnAxis(ap=desti[:, k, i:i + 1], axis=0)))
                                ys.append((yk, yk_T))
                            for k in range(2):
                                yk, yk_T = ys[k]
                                op("dve", lambda e, k=k, yk=yk: e.scalar_tensor_tensor(out=xt[:], in0=yk[:], scalar=GT[:, i, k:k + 1], in1=xt[:],
                                                                                      op0=ALU.mult, op1=ALU.add),
                                   r=[yk_T, xt_T, rt_T], w=[xt_T])
                            ot, ot_T = outr.next()
                            norm_tile(xt[:], xt_T, ot[:], ot_T, smr, junk, junk_T)
                            dma("sp", out_d[i * 128:(i + 1) * 128, :], ot[:], r=[ot_T], w=[T()])
                        barrier()
        except _Stop:
            pass
        S.finish()
        hstack.close()
        print("ops", S.nop, "waits", S.nwait)
    return nc


def _consts():
    c = np.zeros((128, 768), np.float32)
    i = np.arange(128)
    c[:, 0:128] = np.eye(128)
    c[:, 128:256] = (i[:, None] <= i[None, :])
    c[:, 256:384] = (i[None, :] < i[:, None])
    c[:, 384:512] = (i[:, None] < i[None, :])
    c[:, 512:640] = 1.0
    c[:, 640:768] = 1.0 / 256.0
    return c


def _win_perm():
    A_B, A_C, A_X, S_Z, XBC, DT, GLU = 0, 256, 512, 768, 1280, 2304, 2312
    cols = []
    for j in range(2):
        for base in (A_B, A_C, A_X):
            cols += list(range(base + j * 128, base + (j + 1) * 128))
    for j in range(2):
        cols += list(range(GLU + j * 128, GLU + (j + 1) * 128))
        cols += list(range(GLU + 256 + j * 128, GLU + 256 + (j + 1) * 128))
    for g in range(2):
        cols += list(range(XBC + g * 256, XBC + (g + 1) * 256))
        cols += list(range(XBC + 512 + g * 128, XBC + 512 + (g + 1) * 128))
        cols += list(range(XBC + 768 + g * 128, XBC + 768 + (g + 1) * 128))
    cols += list(range(S_Z, S_Z + 512))
    cols += list(range(DT, DT + 8))
    assert len(cols) == IN_COLS and len(set(cols)) == IN_COLS
    return np.array(cols)


def _prep(inp):
    f = lambda a: np.ascontiguousarray(np.asarray(a, dtype=np.float32))
    bcast = lambda v: np.broadcast_to(v, (128,) + v.shape)
    perm = _win_perm()
    d = {}
    d["cst"] = _consts()
    nw = np.stack([inp["norm_mix"][0], inp["norm_ffn"][0], inp["norm_mix"][1], inp["norm_ffn"][1], inp["norm_final"]])
    d["nw"] = f(bcast(nw))
    for l in range(2):
        d[f"win{l}"] = f(inp["w_in"][l][:, perm])
    cwa = np.transpose(inp["conv_a_w"].reshape(2, 3, 2, 128), (3, 0, 2, 1))
    d["cwa"] = f(cwa)
    sw = np.concatenate([inp["conv_ssd_w"], inp["conv_ssd_b"][:, None, :]], axis=1)
    ch = []
    for g in range(2):
        ch += [g * 256 + np.arange(128), g * 256 + 128 + np.arange(128), 512 + g * 128 + np.arange(128), 768 + g * 128 + np.arange(128)]
    ch = np.stack(ch)
    d["cws"] = f(np.transpose(sw[:, :, ch], (3, 0, 2, 1)))
    cc = np.concatenate([inp["conv_conf_w"], inp["conv_conf_b"][:, None, :], inp["conf_ln_g"][:, None, :],
                         inp["conf_ln_b"][:, None, :]], axis=1)
    d["cwc"] = f(np.transpose(cc.reshape(2, 34, 2, 128), (3, 0, 2, 1)))
    hv = np.stack([inp["dt_bias"], inp["a_log"], inp["d_skip"]], axis=1)
    d["hv"] = f(bcast(hv))
    d["snw"] = f(bcast(inp["ssd_norm_w"]))
    d["wout"] = f(inp["w_out"])
    d["ffg"] = f(inp["ffn_w_gate"][0])
    d["ffu"] = f(inp["ffn_w_up"][0])
    d["ffd"] = f(inp["ffn_w_down"][0])
    d["wr"] = f(inp["moe_router"][0])
    d["mg"] = f(inp["moe_w_gate"][0])
    d["mu"] = f(inp["moe_w_up"][0])
    d["md"] = f(inp["moe_w_down"][0])
    return d


_NC = {}


def kernel(**inputs):
    inp = {k: np.asarray(v) for k, v in inputs.items()}
    if "nc" not in _NC:
        _NC["nc"] = build()
    nc = _NC["nc"]
    shared = _prep(inp)
    x = np.ascontiguousarray(inp["x"], dtype=np.float32)
    in_maps = [dict(shared, x=x[b]) for b in range(8)]
    res = run_bass_kernel_spmd(nc, in_maps, core_ids=list(range(8)))
    return np.stack([np.asarray(r["out"]) for r in res.results]).astype(np.float32)
```

```python
import contextlib
import numpy as np
import concourse.bass as bass
import concourse.mybir as mybir
from concourse.bass_utils import run_bass_kernel_spmd

F32 = mybir.dt.float32
BF16 = mybir.dt.bfloat16
I32 = mybir.dt.int32
U32 = mybir.dt.uint32
AF = mybir.ActivationFunctionType
ALU = mybir.AluOpType
AX = mybir.AxisListType
PE_ENG = mybir.EngineType.PE

L = 4096
D = 1024
NT = L // 128
EPS = 1e-5
IN_COLS = 2824
DFF = 2816
NFF = DFF // 128
NE = 8
DFE = 3584
NFE = DFE // 128


class T:
    __slots__ = ("name", "w", "r", "excl")

    def __init__(self, name="", excl=False):
        self.name = name
        self.w = None
        self.r = {}
        self.excl = excl


def TL(n, name=""):
    return [T(f"{name}{i}") for i in range(n)]


class AS:
    def __init__(self, nc, es):
        self.nc = nc
        self.eng = {"pe": nc.tensor, "act": nc.scalar, "dve": nc.vector, "pool": nc.gpsimd, "sp": nc.sync}
        self.sem = {k: es.enter_context(nc.semaphore("s_" + k)) for k in ("pe", "act", "dve", "pool")}
        self.cnt = {k: 0 for k in self.sem}
        self.dq = {}
        for q, n in (("sp", 16), ("pool", 12), ("act", 6)):
            self.dq[q] = {"sems": [es.enter_context(nc.semaphore(f"d_{q}{i}")) for i in range(n)],
                          "cnt": [0] * n, "next": 0}
        self.known = {k: {} for k in self.eng}
        self.snap = {}
        self.nwait = 0
        self.nop = 0
        self.halt = False

    def _semof(self, key):
        if isinstance(key, tuple):
            return self.dq[key[0]]["sems"][key[1]]
        return self.sem[key]

    def _need(self, eng, deps):
        if self.halt:
            return
        kn = self.known[eng]
        for key, val in deps.items():
            if kn.get(key, 0) >= val:
                continue
            if key == eng and eng == "pe":
                continue
            self.eng[eng].wait_ge(self._semof(key), val)
            self.nwait += 1
            kn[key] = val
            sn = self.snap.get((key, val))
            if sn:
                for k2, v2 in sn.items():
                    if kn.get(k2, 0) < v2:
                        kn[k2] = v2

    @staticmethod
    def _deps(reads, writes, eng=None):
        deps = {}

        def add(kv):
            k, v = kv
            if deps.get(k, 0) < v:
                deps[k] = v
        for t in reads:
            if t.w:
                add(t.w)
            if t.excl:
                for kv in t.r.items():
                    if kv[0] != eng:
                        add(kv)
        for t in writes:
            if t.w:
                add(t.w)
            for kv in t.r.items():
                add(kv)
        return deps

    def op(self, eng, fn, r=(), w=()):
        if self.halt:
            return None
        self._need(eng, self._deps(r, w, eng))
        inst = fn(self.eng[eng])
        self.cnt[eng] += 1
        c = self.cnt[eng]
        inst.then_inc(self.sem[eng], 1)
        self.nop += 1
        self.snap[(eng, c)] = dict(self.known[eng])
        for t in r:
            t.r[eng] = c
        for t in w:
            t.w = (eng, c)
            t.r = {}
        return inst

    def dma(self, q, out, in_, r=(), w=(), indirect=None):
        if self.halt:
            return None
        dq = self.dq[q]
        i = dq["next"]
        dq["next"] = (i + 1) % len(dq["sems"])
        key = (q, i)
        deps = self._deps(r, w)
        if dq["cnt"][i] > 0:
            deps[key] = max(deps.get(key, 0), dq["cnt"][i])
        self._need(q, deps)
        e = self.eng[q]
        if indirect is None:
            inst = e.dma_start(out=out, in_=in_)
        else:
            inst = e.indirect_dma_start(out=out, in_=in_, **indirect)
        dq["cnt"][i] += 16
        v = dq["cnt"][i]
        inst.then_inc(dq["sems"][i], 16)
        self.nop += 1
        self.snap[(key, v)] = dict(self.known[q])
        for t in r:
            t.r[key] = v
        for t in w:
            t.w = (key, v)
            t.r = {}
        return inst

    def finish(self):
        self.halt = False
        deps = {}
        for q, dq in self.dq.items():
            for i, c in enumerate(dq["cnt"]):
                if c > 0:
                    deps[(q, i)] = c
        self._need("sp", deps)
        self._need("sp", {k: c for k, c in self.cnt.items() if c > 0})


_UID = [0]


class Ring:
    def __init__(self, nc, es, name, shape, dtype, n):
        _UID[0] += 1
        self.bufs = [es.enter_context(nc.sbuf_tensor(f"r{_UID[0]}_{name}{i}", shape, dtype)) for i in range(n)]
        self.ts = TL(n, name)
        self.i = 0

    def next(self):
        i = self.i
        self.i = (i + 1) % len(self.bufs)
        return self.bufs[i], self.ts[i]


def bc(ap, shape):
    return ap.to_broadcast(shape)


CAP = 12
CAPROWS = CAP * 128
NBLK = NE * CAP
NROW = NBLK * 128


class _Stop(Exception):
    pass


def build(stop_after=99, debug=False):
    nc = bass.Bass("TRN2", target_bir_lowering=False)

    def din(name, shape, dt=F32):
        return nc.dram_tensor(name, shape, dt, kind="ExternalInput").ap()

    def dscr(name, shape, dt=F32):
        kind = "ExternalOutput" if debug else "Internal"
        return nc.dram_tensor(name, shape, dt, kind=kind).ap()

    x_in = din("x", [L, D])
    cst_d = din("cst", [128, 768])
    nw_d = din("nw", [128, 5, D])
    win_d = [din("win0", [D, IN_COLS]), din("win1", [D, IN_COLS])]
    cwa_d = din("cwa", [128, 2, 2, 3])
    cws_d = din("cws", [128, 2, 8, 5])
    cwc_d = din("cwc", [128, 2, 2, 34])
    hv_d = din("hv", [128, 2, 3, 8])
    snw_d = din("snw", [128, 2, 512])
    wout_d = din("wout", [2, D, D])
    ffg_d = din("ffg", [D, DFF])
    ffu_d = din("ffu", [D, DFF])
    ffd_d = din("ffd", [DFF, D])
    wr_d = din("wr", [D, NE])
    need_moe = stop_after > 12
    if need_moe:
        mg_d = din("mg", [NE, D, DFE])
        mu_d = din("mu", [NE, D, DFE])
        md_d = din("md", [NE, DFE, D])
    else:
        mg_d = nc.dram_tensor("mg", [NE, D, DFE], F32, kind="Internal").ap()
        mu_d = nc.dram_tensor("mu", [NE, D, DFE], F32, kind="Internal").ap()
        md_d = nc.dram_tensor("md", [NE, DFE, D], F32, kind="Internal").ap()
    out_d = nc.dram_tensor("out", [L, D], F32, kind="ExternalOutput").ap()

    xres_d = dscr("xres", [L, D])
    yT_d = dscr("yT", [D, L], BF16)
    hm_d = nc.dram_tensor("hm", [NROW + 128, D], BF16, kind="Internal").ap()
    yb_d = nc.dram_tensor("yb", [NROW + 128, D], F32, kind="Internal").ap()

    xres_T = TL(NT, "xres")
    yT_T = TL(8, "yT")

    with contextlib.ExitStack() as es:
        S = AS(nc, es)
        op, dma = S.op, S.dma

        def sb(name, shape, dt=F32, stack=es):
            _UID[0] += 1
            return stack.enter_context(nc.sbuf_tensor(f"t{_UID[0]}_{name}", shape, dt))

        cst = sb("cst", [128, 768]); cst_T = T("cst")
        identb = sb("identb", [128, 128], BF16); identb_T = T("identb")
        nwb = sb("nwb", [128, D]); nwb_T = T("nwb")
        epsc = sb("epsc", [128, 1]); epsc_T = T("epsc")
        PSF = [es.enter_context(nc.psum_tensor(f"psf{i}", [128, 512], F32)) for i in range(6)]
        PSB = [es.enter_context(nc.psum_tensor(f"psb{i}", [128, 1024], BF16)) for i in range(2)]
        PSF_T = [T(f"psf{i}", excl=True) for i in range(6)]
        PSB_T = [T(f"psb{i}", excl=True) for i in range(2)]
        GT = sb("m_GT", [128, NT, 2], F32)
        desti = sb("m_desti", [128, 2, NT], U32)
        rt_T = T("route")
        hstack = contextlib.ExitStack()
        HT = sb("HT", [128, 8, L], BF16, hstack); HT_T = TL(NT, "HT")

        ident = cst[:, 0:128]
        LEm = cst[:, 128:256]
        LTs = cst[:, 256:384]
        SUm = cst[:, 384:512]
        ones = cst[:, 512:640]
        ones256 = cst[:, 640:768]

        op("dve", lambda e: e.memset(epsc[:], EPS), w=[epsc_T])
        dma("sp", cst[:], cst_d[:], w=[cst_T])
        op("dve", lambda e: e.tensor_copy(out=identb[:], in_=ident), r=[cst_T], w=[identb_T])

        def acopy(out, in_, r, w, eng="act"):
            if eng == "act":
                op("act", lambda e: e.activation(out=out, in_=in_, func=AF.Copy), r=r, w=w)
            else:
                op(eng, lambda e: e.tensor_copy(out=out, in_=in_), r=r, w=w)

        def barrier():
            allc = {k: c for k, c in S.cnt.items() if c > 0}
            for q, dq in S.dq.items():
                for i, c in enumerate(dq["cnt"]):
                    if c > 0:
                        allc[(q, i)] = c
            for e_ in ("pe", "act", "dve", "pool", "sp"):
                S._need(e_, allc)

        def rstd_from_ss(ss, n, tmp, rs, t_):
            op("act", lambda e: e.activation(out=tmp, in_=ss, func=AF.Ln, scale=1.0 / n, bias=epsc[:, 0:1]),
               r=[t_, epsc_T], w=[t_])
            op("act", lambda e: e.activation(out=rs, in_=tmp, func=AF.Exp, scale=-0.5), r=[t_], w=[t_])

        def norm_tile(xt, xt_T, out, out_T, smr, junk, junk_T):
            sm, sm_T = smr.next()
            op("act", lambda e: e.activation(out=junk[:], in_=xt, func=AF.Square, accum_out=sm[:, 0:1]),
               r=[xt_T], w=[junk_T, sm_T])
            rstd_from_ss(sm[:, 0:1], D, sm[:, 1:2], sm[:, 2:3], sm_T)
            op("dve", lambda e: e.scalar_tensor_tensor(out=out, in0=xt, scalar=sm[:, 2:3], in1=nwb[:],
                                                       op0=ALU.mult, op1=ALU.mult),
               r=[xt_T, sm_T, nwb_T], w=[out_T])

        def to_HT(hn, hn_T, i):
            pb, pb_T = PSB[i % 2], PSB_T[i % 2]
            for k in range(8):
                op("pe", lambda e, k=k: e.transpose(out=pb[:, k * 128:(k + 1) * 128], in_=hn[:, k * 128:(k + 1) * 128],
                                                    identity=identb[:]),
                   r=[hn_T, identb_T], w=[pb_T])
            acopy(HT[:, :, i * 128:(i + 1) * 128], pb[:].rearrange("p (k t) -> p k t", k=8), [pb_T], [HT_T[i]])

        def stop_at(x):
            if stop_after <= x:
                S.halt = True

        try:
            for layer in range(2):
                res_src = x_in if layer == 0 else xres_d
                if layer == 0:
                    with contextlib.ExitStack() as ps:
                        xin = Ring(nc, ps, "a_x", [128, D], F32, 3)
                        hnr = Ring(nc, ps, "a_hn", [128, D], BF16, 2)
                        smr = Ring(nc, ps, "a_sm", [128, 4], F32, 3)
                        junk = sb("a_junk", [128, D], BF16, ps); junk_T = T()
                        dma("sp", nwb[:], nw_d[:, 0, :], w=[nwb_T])
                        for i in range(NT):
                            xt, xt_T = xin.next()
                            dma("sp", xt[:], x_in[i * 128:(i + 1) * 128, :], w=[xt_T])
                            hn, hn_T = hnr.next()
                            norm_tile(xt[:], xt_T, hn[:], hn_T, smr, junk, junk_T)
                            to_HT(hn, hn_T, i)
                        barrier()
                stop_at(0)

                with contextlib.ExitStack() as ps:
                    FB = [sb(f"b_F{i}", [128, L + 32], F32, ps) for i in range(3)]
                    FB_T = TL(3, "F")
                    HB = [sb(f"b_H{i}", [128, L], BF16, ps) for i in range(4)]
                    HB_T = TL(4, "H")
                    wch = Ring(nc, ps, "b_w", [128, 8, 512], BF16, 2)
                    cwa = sb("b_cwa", [128, 2, 3], F32, ps); cwa_T = T()
                    cws = sb("b_cws", [128, 8, 5], F32, ps); cws_T = T()
                    cwc = sb("b_cwc", [128, 2, 34], F32, ps); cwc_T = T()
                    hv = sb("b_hv", [128, 3, 8], F32, ps); hv_T = T()
                    snw = sb("b_snw", [128, 512], F32, ps); snw_T = T()
                    acs = contextlib.ExitStack()
                    ps.callback(acs.close)
                    tmpf = Ring(nc, acs, "b_tmpf", [128, 512], F32, 4)
                    gpb = sb("b_gpb", [128, L + 32], BF16, acs); gpb_T = T()
                    dgw = sb("b_dgw", [128, 31, 128], BF16, acs); dgw_T = T()
                    op("pool", lambda e: e.memset(gpb[:, 0:32], 0.0), w=[gpb_T])
                    dma("sp", cwa[:], cwa_d[:, layer], w=[cwa_T])
                    dma("sp", cws[:], cws_d[:, layer], w=[cws_T])
                    dma("sp", cwc[:], cwc_d[:, layer], w=[cwc_T])
                    dma("sp", hv[:], hv_d[:, layer], w=[hv_T])
                    dma("sp", snw[:], snw_d[:, layer], w=[snw_T])
                    for f in range(3):
                        op("pool", lambda e, f=f: e.memset(FB[f][:, 0:32], 0.0), w=[FB_T[f]])
                    win = win_d[layer].rearrange("(kc p) c -> p kc c", p=128)
                    psrot = [0]

                    def load_w(col0, nch):
                        wt, wt_T = wch.next()
                        dma("pool", wt[:, :, 0:nch * 128], win[:, :, col0:col0 + nch * 128], w=[wt_T])
                        return [(wt[:, :, k * 128:(k + 1) * 128], wt_T) for k in range(nch)]

                    def proj_blk(wt, wt_T, blk):
                        bi = psrot[0]
                        psrot[0] = (bi + 1) % 4
                        p_, p_T = PSF[bi], PSF_T[bi]
                        rT = [wt_T] + HT_T[blk * 4:(blk + 1) * 4]
                        for kc in range(8):
                            op("pe", lambda e, kc=kc: e.matmul(p_[:], lhsT=wt[:, kc, :], rhs=HT[:, kc, blk * 512:(blk + 1) * 512],
                                                               start=(kc == 0), stop=(kc == 7)), r=rT, w=[p_T])
                        return p_, p_T

                    def conv_taps(src, src_T, K, wts, wT, acc, acc_T, bias=None, step=2048):
                        for s0 in range(0, L, step):
                            n = step
                            for j in range(K):
                                o = 32 - (K - 1) + j + s0
                                if j == 0:
                                    if bias is None:
                                        op("dve", lambda e, o=o, s0=s0: e.tensor_scalar(
                                            out=acc[:, s0:s0 + n], in0=src[:, o:o + n], scalar1=wts[:, 0:1], scalar2=None,
                                            op0=ALU.mult), r=[src_T, wT], w=[acc_T])
                                    else:
                                        op("dve", lambda e, o=o, s0=s0: e.tensor_scalar(
                                            out=acc[:, s0:s0 + n], in0=src[:, o:o + n], scalar1=wts[:, 0:1], scalar2=bias,
                                            op0=ALU.mult, op1=ALU.add), r=[src_T, wT], w=[acc_T])
                                else:
                                    op("dve", lambda e, o=o, s0=s0, j=j: e.scalar_tensor_tensor(
                                        out=acc[:, s0:s0 + n], in0=src[:, o:o + n], scalar=wts[:, j:j + 1], in1=acc[:, s0:s0 + n],
                                        op0=ALU.mult, op1=ALU.add), r=[src_T, wT, acc_T], w=[acc_T])

                    for j in range(2):
                        (wb_, wb_T), (wc_, wc_T), (wx_, wx_T) = load_w(j * 384, 3)
                        for blk in range(8):
                            sl = slice(blk * 512, (blk + 1) * 512)
                            slp = slice(32 + blk * 512, 32 + (blk + 1) * 512)
                            pc, pc_T = proj_blk(wc_, wc_T, blk)
                            tf, tf_T = tmpf.next()
                            acopy(tf[:], pc[:], [pc_T], [tf_T])
                            px, px_T = proj_blk(wx_, wx_T, blk)
                            op("dve", lambda e: e.tensor_tensor(out=FB[0][:, slp], in0=tf[:], in1=px[:], op=ALU.mult),
                               r=[tf_T, px_T], w=[FB_T[0]])
                            pb_, pb_T = proj_blk(wb_, wb_T, blk)
                            acopy(FB[1][:, sl], pb_[:], [pb_T], [FB_T[1]])
                        conv_taps(FB[0], FB_T[0], 3, cwa[:, j, :], cwa_T, FB[2], FB_T[2])
                        for s0 in range(0, L, 2048):
                            op("dve", lambda e, s0=s0: e.tensor_tensor(out=HB[j][:, s0:s0 + 2048], in0=FB[2][:, s0:s0 + 2048],
                                                                       in1=FB[1][:, s0:s0 + 2048], op=ALU.mult),
                               r=[FB_T[2], FB_T[1]], w=[HB_T[j]])
                        dma("sp", yT_d[j * 128:(j + 1) * 128, :], HB[j][:], r=[HB_T[j]], w=[yT_T[j]])

                    if layer == 0:
                        stop_at(0.2)
                    for j in range(2):
                        (wa_, wa_T), (wg_, wg_T) = load_w(768 + j * 256, 2)
                        for blk in range(8):
                            slp = slice(32 + blk * 512, 32 + (blk + 1) * 512)
                            pg, pg_T = proj_blk(wg_, wg_T, blk)
                            tf, tf_T = tmpf.next()
                            op("act", lambda e: e.activation(out=tf[:], in_=pg[:], func=AF.Sigmoid), r=[pg_T], w=[tf_T])
                            pa, pa_T = proj_blk(wa_, wa_T, blk)
                            op("dve", lambda e: e.tensor_tensor(out=gpb[:, slp], in0=tf[:], in1=pa[:], op=ALU.mult),
                               r=[tf_T, pa_T], w=[gpb_T])
                        for t_ in range(31):
                            op("dve", lambda e, t_=t_: e.tensor_scalar(out=dgw[:, t_, :], in0=identb[:], scalar1=cwc[:, j, t_:t_ + 1],
                                                                       scalar2=None, op0=ALU.mult), r=[identb_T, cwc_T], w=[dgw_T])
                        for blk in range(8):
                            bi = psrot[0]
                            psrot[0] = (bi + 1) % 4
                            for t_ in range(31):
                                o = 32 - 30 + t_ + blk * 512
                                op("pe", lambda e, t_=t_, o=o: e.matmul(PSF[bi][:], lhsT=dgw[:, t_, :], rhs=gpb[:, o:o + 512],
                                                                        start=(t_ == 0), stop=(t_ == 30)), r=[dgw_T, gpb_T], w=[PSF_T[bi]])
                            op("act", lambda e, blk=blk: e.activation(out=FB[1 + j][:, blk * 512:(blk + 1) * 512], in_=PSF[bi][:],
                                                                      func=AF.Identity, bias=cwc[:, j, 31:32]),
                               r=[PSF_T[bi], cwc_T], w=[FB_T[1 + j]])
                    for blk in range(8):
                        sl = slice(blk * 512, (blk + 1) * 512)
                        sq = []
                        for j in range(2):
                            tf, tf_T = tmpf.next()
                            op("act", lambda e, j=j: e.activation(out=tf[:], in_=FB[1 + j][:, sl], func=AF.Square),
                               r=[FB_T[1 + j]], w=[tf_T])
                            sq.append((tf, tf_T))
                        pm, pm_T = PSF[4], PSF_T[4]
                        pe2, pe2_T = PSF[5], PSF_T[5]
                        for j in range(2):
                            op("pe", lambda e, j=j: e.matmul(pm[:], lhsT=ones256, rhs=FB[1 + j][:, sl], start=(j == 0), stop=(j == 1)),
                               r=[cst_T, FB_T[1 + j]], w=[pm_T])
                        for j in range(2):
                            op("pe", lambda e, j=j: e.matmul(pe2[:], lhsT=ones256, rhs=sq[j][0][:], start=(j == 0), stop=(j == 1)),
                               r=[cst_T, sq[j][1]], w=[pe2_T])
                        mean, mean_T = tmpf.next()
                        acopy(mean[:], pm[:], [pm_T], [mean_T])
                        var, var_T = sq[0]
                        op("dve", lambda e: e.tensor_tensor(out=var[:], in0=mean[:], in1=mean[:], op=ALU.mult), r=[mean_T], w=[var_T])
                        op("dve", lambda e: e.tensor_tensor(out=var[:], in0=pe2[:], in1=var[:], op=ALU.subtract),
                           r=[pe2_T, var_T], w=[var_T])
                        op("dve", lambda e: e.tensor_scalar(out=var[:], in0=var[:], scalar1=EPS, scalar2=None, op0=ALU.add),
                           r=[var_T], w=[var_T])
                        op("act", lambda e: e.activation(out=var[:], in_=var[:], func=AF.Sqrt), r=[var_T], w=[var_T])
                        op("dve", lambda e: e.reciprocal(out=var[:], in_=var[:]), r=[var_T], w=[var_T])
                        t2, t2_T = sq[1]
                        for j in range(2):
                            op("dve", lambda e, j=j: e.tensor_tensor(out=t2[:], in0=FB[1 + j][:, sl], in1=mean[:], op=ALU.subtract),
                               r=[FB_T[1 + j], mean_T], w=[t2_T])
                            op("dve", lambda e: e.tensor_tensor(out=t2[:], in0=t2[:], in1=var[:], op=ALU.mult),
                               r=[t2_T, var_T], w=[t2_T])
                            op("act", lambda e, j=j: e.activation(out=HB[2 + j][:, sl], in_=t2[:], func=AF.Silu,
                                                                  scale=cwc[:, j, 32:33], bias=cwc[:, j, 33:34]),
                               r=[t2_T, cwc_T], w=[HB_T[2 + j]])
                    for j in range(2):
                        dma("sp", yT_d[768 + j * 128:768 + (j + 1) * 128, :], HB[2 + j][:], r=[HB_T[2 + j]], w=[yT_T[6 + j]])

                    if layer == 0:
                        stop_at(0.4)
                    barrier()
                    acs.close()
                    with contextlib.ExitStack() as ss_:
                        rYst = Ring(nc, ss_, "s_yst", [128, 2, 256], BF16, 2)
                        wzz = sb("s_wz", [128, 8, 520], BF16, ss_); wz_T = T()
                        dma("pool", wzz[:], win[:, :, 2304:2824], w=[wz_T])
                        wdt_T = wz_T
                        dtt = sb("s_dt", [128, NT, 8], F32, ss_); dtt_T = T()
                        dtA = sb("s_dtA", [128, NT, 8], F32, ss_); dtA_T = T()
                        acum = sb("s_acum", [128, NT, 8], F32, ss_); acum_T = T()
                        Eall = sb("s_E", [128, NT, 8], F32, ss_); Eall_T = T()
                        cdall = sb("s_cd", [128, NT, 8], F32, ss_); cdall_T = T()
                        dte = sb("s_dte", [128, NT, 8], F32, ss_); dte_T = T()
                        ea = sb("s_ea", [128, 8], F32, ss_); ea_T = T()
                        hs = sb("s_hs", [128, 4, 64], F32, ss_); hs_T = T()
                        hbf = sb("s_hbf", [128, 256], BF16, ss_); hbf_T = T()
                        rG = Ring(nc, ss_, "s_G", [128, 128], F32, 2)
                        rR4 = Ring(nc, ss_, "s_r4", [128, 4, 128], F32, 1)
                        rEx = Ring(nc, ss_, "s_ex", [128, 4, 128], F32, 1)
                        rM = Ring(nc, ss_, "s_M", [128, 4, 128], BF16, 2)
                        rXs = Ring(nc, ss_, "s_xs", [128, 4, 64], BF16, 2)
                        rXd = Ring(nc, ss_, "s_xd", [128, 4, 64], BF16, 2)
                        rXd2 = Ring(nc, ss_, "s_xd2", [128, 4, 64], BF16, 2)
                        rBt = Ring(nc, ss_, "s_bt", [128, 128], BF16, 2)
                        rT1 = Ring(nc, ss_, "s_t1", [128, 4, 64], F32, 2)
                        rT3 = Ring(nc, ss_, "s_t3", [128, 4, 64], F32, 1)
                        rSz = Ring(nc, ss_, "s_sz", [128, 256], F32, 3)
                        rYo = Ring(nc, ss_, "s_yo", [128, 256], BF16, 2)
                        rSm = Ring(nc, ss_, "s_sm", [128, 4], F32, 3)
                        junk = sb("s_junk", [128, 256], BF16, ss_); junk_T = T()

                        pdt, pdt_T = PSF[4], PSF_T[4]
                        for i in range(NT):
                            for kc in range(8):
                                op("pe", lambda e, kc=kc, i=i: e.matmul(pdt[:, i * 8:(i + 1) * 8], lhsT=HT[:, kc, i * 128:(i + 1) * 128],
                                                                        rhs=wzz[:, kc, 512:520], start=(kc == 0), stop=(kc == 7)),
                                   r=[HT_T[i], wdt_T], w=[pdt_T])
                        pdt3 = pdt[:, 0:256].rearrange("p (c h) -> p c h", h=8)
                        op("dve", lambda e: e.tensor_tensor(out=dtt[:], in0=pdt3, in1=bc(hv[:, 0:1, :], [128, NT, 8]), op=ALU.add),
                           r=[pdt_T, hv_T], w=[dtt_T])
                        op("act", lambda e: e.activation(out=dtt[:], in_=dtt[:], func=AF.Exp), r=[dtt_T], w=[dtt_T])
                        op("dve", lambda e: e.tensor_scalar(out=dtt[:], in0=dtt[:], scalar1=1.0, scalar2=None, op0=ALU.add),
                           r=[dtt_T], w=[dtt_T])
                        op("act", lambda e: e.activation(out=dtt[:], in_=dtt[:], func=AF.Ln), r=[dtt_T], w=[dtt_T])
                        if layer == 0:
                            stop_at(0.5)
                        op("act", lambda e: e.activation(out=ea[:], in_=hv[:, 1, :], func=AF.Exp), r=[hv_T], w=[ea_T])
                        op("dve", lambda e: e.scalar_tensor_tensor(out=dtA[:], in0=dtt[:], scalar=-1.0,
                                                                   in1=bc(ea[:].rearrange("p (o h) -> p o h", o=1), [128, NT, 8]),
                                                                   op0=ALU.mult, op1=ALU.mult), r=[dtt_T, ea_T], w=[dtA_T])
                        fl = lambda t_: t_[:].rearrange("p c h -> p (c h)")
                        pac, pac_T = PSF[5], PSF_T[5]
                        op("pe", lambda e: e.matmul(pac[:, 0:256], lhsT=LEm, rhs=fl(dtA), start=True, stop=True),
                           r=[cst_T, dtA_T], w=[pac_T])
                        op("pe", lambda e: e.matmul(pac[:, 256:512], lhsT=ones, rhs=fl(dtA), start=True, stop=True),
                           r=[cst_T, dtA_T], w=[pac_T])
                        acopy(fl(acum), pac[:, 0:256], [pac_T], [acum_T])
                        op("act", lambda e: e.activation(out=fl(Eall), in_=pac[:, 0:256], func=AF.Exp), r=[pac_T], w=[Eall_T])
                        op("act", lambda e: e.activation(out=fl(cdall), in_=pac[:, 256:512], func=AF.Exp), r=[pac_T], w=[cdall_T])
                        op("dve", lambda e: e.tensor_tensor(out=fl(dte), in0=pac[:, 256:512], in1=fl(acum), op=ALU.subtract),
                           r=[pac_T, acum_T], w=[dte_T])
                        op("act", lambda e: e.activation(out=fl(dte), in_=fl(dte), func=AF.Exp), r=[dte_T], w=[dte_T])

                        if layer == 0:
                            stop_at(0.6)

                        def hb(t_, c, g):
                            return t_[:, c:c + 1, g * 4:(g + 1) * 4].rearrange("p o h -> p h o")

                        for g in range(2):
                            cb = 1280 + g * 512
                            wqs = load_w(cb, 4)
                            for q in range(4):
                                wq, wq_T = wqs[q]
                                fpad_T = FB_T[q % 2]
                                fpad = FB[q % 2][:].bitcast(BF16)
                                dg_T = FB_T[2]
                                dgv = FB[2][:].bitcast(BF16)
                                ci = g * 4 + q
                                op("pool", lambda e: e.memset(fpad[:, 0:32], 0.0), w=[fpad_T])
                                for t_ in range(4):
                                    op("dve", lambda e, t_=t_: e.tensor_scalar(out=dgv[:, (q * 4 + t_) * 128:(q * 4 + t_ + 1) * 128], in0=identb[:],
                                                                               scalar1=cws[:, ci, t_:t_ + 1], scalar2=None, op0=ALU.mult),
                                       r=[identb_T, cws_T], w=[dg_T])
                                for blk in range(8):
                                    pq, pq_T = proj_blk(wq, wq_T, blk)
                                    acopy(fpad[:, 32 + blk * 512:32 + (blk + 1) * 512], pq[:], [pq_T], [fpad_T])
                                for blk in range(8):
                                    bi = psrot[0]
                                    psrot[0] = (bi + 1) % 4
                                    for t_ in range(4):
                                        o = 32 - 3 + t_ + blk * 512
                                        op("pe", lambda e, t_=t_, o=o: e.matmul(PSF[bi][:], lhsT=dgv[:, (q * 4 + t_) * 128:(q * 4 + t_ + 1) * 128],
                                                                                rhs=fpad[:, o:o + 512], start=(t_ == 0), stop=(t_ == 3)),
                                           r=[dg_T, fpad_T], w=[PSF_T[bi]])
                                    op("act", lambda e, blk=blk, q=q: e.activation(out=HB[q][:, blk * 512:(blk + 1) * 512], in_=PSF[bi][:],
                                                                                  func=AF.Silu, bias=cws[:, ci, 4:5]),
                                       r=[PSF_T[bi], cws_T], w=[HB_T[q]])
                            if layer == 0 and g == 0:
                                stop_at(0.7)
                            op("dve", lambda e: e.memset(hs[:], 0.0), w=[hs_T])
                            op("dve", lambda e: e.memset(hbf[:], 0.0), w=[hbf_T])
                            def front(c):
                                sl = slice(c * 128, (c + 1) * 128)
                                op("pe", lambda e: e.matmul(PSF[1][:, 0:128], lhsT=HB[2][:, sl], rhs=HB[3][:, sl], start=True, stop=True),
                                   r=[HB_T[2], HB_T[3]], w=[PSF_T[1]])
                                G, G_T = rG.next()
                                op("dve", lambda e: e.tensor_tensor(out=G[:], in0=PSF[1][:, 0:128], in1=LEm, op=ALU.mult),
                                   r=[PSF_T[1], cst_T], w=[G_T])
                                r4, r4_T = rR4.next()
                                op("dve", lambda e: e.tensor_tensor(out=r4[:], in0=bc(LEm.rearrange("p (o l) -> p o l", o=1), [128, 4, 128]),
                                                                    in1=bc(hb(dtA, c, g), [128, 4, 128]), op=ALU.mult),
                                   r=[cst_T, dtA_T], w=[r4_T])
                                sgi = 0 if c % 2 == 0 else 5
                                op("pe", lambda e: e.matmul(PSF[sgi][:], lhsT=LTs, rhs=r4[:].rearrange("p h l -> p (h l)"),
                                                            start=True, stop=True), r=[cst_T, r4_T], w=[PSF_T[sgi]])
                                ex, ex_T = rEx.next()
                                op("act", lambda e: e.activation(out=ex[:].rearrange("p h l -> p (h l)"), in_=PSF[sgi][:], func=AF.Exp),
                                   r=[PSF_T[sgi]], w=[ex_T])
                                M, M_T = rM.next()
                                op("pool", lambda e: e.tensor_tensor(out=M[:], in0=ex[:],
                                                                     in1=bc(G[:].rearrange("p (o l) -> p o l", o=1), [128, 4, 128]),
                                                                     op=ALU.mult), r=[ex_T, G_T], w=[M_T])
                                for q in range(2):
                                    op("pe", lambda e, q=q: e.transpose(out=PSB[0][:, q * 128:(q + 1) * 128], in_=HB[q][:, sl],
                                                                        identity=identb[:]), r=[HB_T[q], identb_T], w=[PSB_T[0]])
                                xs, xs_T = rXs.next()
                                px3 = PSB[0][:, 0:256].rearrange("p (h d) -> p h d", h=4)
                                acopy(xs[:], px3, [PSB_T[0]], [xs_T])
                                xd, xd_T = rXd.next()
                                op("dve", lambda e: e.tensor_tensor(out=xd[:], in0=px3, in1=bc(hb(dtt, c, g), [128, 4, 64]), op=ALU.mult),
                                   r=[PSB_T[0], dtt_T], w=[xd_T])
                                xd2, xd2_T = rXd2.next()
                                op("pool", lambda e: e.tensor_tensor(out=xd2[:], in0=xd[:], in1=bc(hb(dte, c, g), [128, 4, 64]), op=ALU.mult),
                                   r=[xd_T, dte_T], w=[xd2_T])
                                op("pe", lambda e: e.transpose(out=PSB[0][:, 256:384], in_=HB[2][:, sl], identity=identb[:]),
                                   r=[HB_T[2], identb_T], w=[PSB_T[0]])
                                bt, bt_T = rBt.next()
                                acopy(bt[:], PSB[0][:, 256:384], [PSB_T[0]], [bt_T])
                                for kc in range(8):
                                    op("pe", lambda e, kc=kc: e.matmul(PSF[4][:, 0:256], lhsT=HT[:, kc, sl], rhs=wzz[:, kc, g * 256:(g + 1) * 256],
                                                                       start=(kc == 0), stop=(kc == 7)), r=[HT_T[c], wz_T], w=[PSF_T[4]])
                                sz, sz_T = rSz.next()
                                op("act", lambda e: e.activation(out=sz[:], in_=PSF[4][:, 0:256], func=AF.Exp, scale=-1.0),
                                   r=[PSF_T[4]], w=[sz_T])
                                op("dve", lambda e: e.tensor_scalar(out=sz[:], in0=sz[:], scalar1=1.0, scalar2=None, op0=ALU.add),
                                   r=[sz_T], w=[sz_T])
                                op("dve", lambda e: e.reciprocal(out=sz[:], in_=sz[:]), r=[sz_T], w=[sz_T])
                                op("dve", lambda e: e.tensor_tensor(out=sz[:], in0=sz[:], in1=PSF[4][:, 0:256], op=ALU.mult),
                                   r=[sz_T, PSF_T[4]], w=[sz_T])
                                return (M, M_T, xs, xs_T, xd, xd_T, xd2, xd2_T, bt, bt_T, sz, sz_T)

                            def back(c, P):
                                M, M_T, xs, xs_T, xd, xd_T, xd2, xd2_T, bt, bt_T, sz, sz_T = P
                                sl = slice(c * 128, (c + 1) * 128)
                                for r_ in range(4):
                                    op("pe", lambda e, r_=r_: e.matmul(PSF[2][:, r_ * 64:(r_ + 1) * 64], lhsT=M[:, r_, :], rhs=xd[:, r_, :],
                                                                       start=True, stop=True), r=[M_T, xd_T], w=[PSF_T[2]])
                                op("pe", lambda e: e.matmul(PSF[3][:, 0:256], lhsT=HB[3][:, sl], rhs=hbf[:], start=True, stop=True),
                                   r=[HB_T[3], hbf_T], w=[PSF_T[3]])
                                op("pe", lambda e: e.matmul(PSF[3][:, 256:512], lhsT=bt[:], rhs=xd2[:].rearrange("p h d -> p (h d)"),
                                                            start=True, stop=True), r=[bt_T, xd2_T], w=[PSF_T[3]])
                                t1, t1_T = rT1.next()
                                op("dve", lambda e: e.tensor_tensor(out=t1[:], in0=PSF[3][:, 0:256].rearrange("p (h d) -> p h d", h=4),
                                                                    in1=bc(hb(Eall, c, g), [128, 4, 64]), op=ALU.mult),
                                   r=[PSF_T[3], Eall_T], w=[t1_T])
                                op("dve", lambda e: e.tensor_tensor(out=t1[:], in0=t1[:],
                                                                    in1=PSF[2][:, 0:256].rearrange("p (h d) -> p h d", h=4), op=ALU.add),
                                   r=[t1_T, PSF_T[2]], w=[t1_T])
                                t3, t3_T = rT3.next()
                                op("pool", lambda e: e.tensor_tensor(out=t3[:], in0=xs[:],
                                                                     in1=bc(hv[:, 2:3, g * 4:(g + 1) * 4].rearrange("p o h -> p h o"), [128, 4, 64]),
                                                                     op=ALU.mult), r=[xs_T, hv_T], w=[t3_T])
                                op("dve", lambda e: e.tensor_tensor(out=t1[:], in0=t1[:], in1=t3[:], op=ALU.add), r=[t1_T, t3_T], w=[t1_T])
                                op("dve", lambda e: e.tensor_tensor(out=hs[:], in0=hs[:], in1=bc(hb(cdall, c, g), [128, 4, 64]), op=ALU.mult),
                                   r=[hs_T, cdall_T], w=[hs_T])
                                op("dve", lambda e: e.tensor_tensor(out=hs[:], in0=hs[:],
                                                                    in1=PSF[3][:, 256:512].rearrange("p (h d) -> p h d", h=4), op=ALU.add),
                                   r=[hs_T, PSF_T[3]], w=[hs_T])
                                acopy(hbf[:], hs[:].rearrange("p h d -> p (h d)"), [hs_T], [hbf_T])
                                return (t1, t1_T, sz, sz_T)

                            def back2(c, Q):
                                t1, t1_T, sz, sz_T = Q
                                sl = slice(c * 128, (c + 1) * 128)
                                op("dve", lambda e: e.tensor_tensor(out=sz[:], in0=sz[:], in1=t1[:].rearrange("p h d -> p (h d)"), op=ALU.mult),
                                   r=[sz_T, t1_T], w=[sz_T])
                                sm, sm_T = rSm.next()
                                op("act", lambda e: e.activation(out=junk[:], in_=sz[:], func=AF.Square, accum_out=sm[:, 0:1]),
                                   r=[sz_T], w=[junk_T, sm_T])
                                rstd_from_ss(sm[:, 0:1], 256, sm[:, 1:2], sm[:, 2:3], sm_T)
                                yo, yo_T = rYo.next()
                                op("dve", lambda e: e.scalar_tensor_tensor(out=yo[:], in0=sz[:], scalar=sm[:, 2:3],
                                                                           in1=snw[:, g * 256:(g + 1) * 256], op0=ALU.mult, op1=ALU.mult),
                                   r=[sz_T, sm_T, snw_T], w=[yo_T])
                                for q in range(2):
                                    op("pe", lambda e, q=q: e.transpose(out=PSB[1][:, q * 128:(q + 1) * 128], in_=yo[:, q * 128:(q + 1) * 128],
                                                                        identity=identb[:]), r=[yo_T, identb_T], w=[PSB_T[1]])
                                if c % 2 == 0:
                                    ystate["y"] = rYst.next()
                                yst, yst_T = ystate["y"]
                                acopy(yst[:, :, (c % 2) * 128:(c % 2 + 1) * 128], PSB[1][:, 0:256].rearrange("p (q t) -> p q t", q=2),
                                      [PSB_T[1]], [yst_T])
                                if c % 2 == 1:
                                    for q in range(2):
                                        r0 = 256 + g * 256 + q * 128
                                        dma("sp", yT_d[r0:r0 + 128, (c - 1) * 128:(c + 1) * 128], yst[:, q, :], r=[yst_T],
                                            w=[yT_T[2 + g * 2 + q]] if c == NT - 1 else [T()])

                            ystate = {}
                            P_ = front(0)
                            Qp = None
                            for c in range(NT):
                                Pn = front(c + 1) if c + 1 < NT else None
                                Q_ = back(c, P_)
                                if Qp is not None:
                                    back2(c - 1, Qp)
                                Qp = Q_
                                P_ = Pn
                            back2(NT - 1, Qp)
                        barrier()
                    barrier()
                stop_at(1 + 10 * layer)

                moe = (layer == 1)
                if moe:
                    hstack.close()
                with contextlib.ExitStack() as ps:
                    wo = sb("c_wo", [128, 8, D], BF16, ps); wo_T = T()
                    dma("pool", wo[:], wout_d[layer].rearrange("(cc p) d -> p cc d", p=128), w=[wo_T])
                    ytl = Ring(nc, ps, "c_y", [128, 8, 512], BF16, 2)
                    xin = Ring(nc, ps, "c_x", [128, D], F32, 3)
                    smr = Ring(nc, ps, "c_sm", [128, 4], F32, 3)
                    junk = sb("c_junk", [128, D], BF16, ps); junk_T = T()
                    dma("sp", nwb[:], nw_d[:, 1 + 2 * layer, :], w=[nwb_T])
                    yT_v = yT_d.rearrange("(cc p) t -> p cc t", p=128)
                    if not moe:
                        hnr = Ring(nc, ps, "c_hn", [128, D], BF16, 2)
                    else:
                        hn32 = Ring(nc, ps, "c_h32", [128, D], F32, 2)
                        h3T = Ring(nc, ps, "c_h3T", [128, 8, 128], F32, 2)
                        H3 = sb("m_H3", [128, NT, D], BF16, ps); H3_T = TL(NT, "H3")
                        wr = sb("m_wr", [128, 8, NE], F32, ps); wr_T = T()
                        dma("sp", wr[:], wr_d.rearrange("(kc p) e -> p kc e", p=128), w=[wr_T])
                        lgr = Ring(nc, ps, "m_lg", [128, 24], F32, 3)
                        M1m = sb("m_M1", [128, NT, NE], F32, ps)
                        M2m = sb("m_M2", [128, NT, NE], F32, ps)
                        RK = sb("m_RK", [128, NT, NE], F32, ps)
                        tot = sb("m_tot", [128, NE], F32, ps); tot_T = T()
                        op("dve", lambda e: e.memset(tot[:], 0.0), w=[tot_T])
                    for blk in range(8):
                        yl, yl_T = ytl.next()
                        dma("sp", yl[:], yT_v[:, :, blk * 512:(blk + 1) * 512], r=yT_T, w=[yl_T])
                        for s in range(4):
                            i = blk * 4 + s
                            xt, xt_T = xin.next()
                            dma("sp", xt[:], res_src[i * 128:(i + 1) * 128, :], r=([xres_T[i]] if layer else []), w=[xt_T])
                            for dh in range(2):
                                bi = (2 * i + dh) % 4
                                for cc in range(8):
                                    op("pe", lambda e, cc=cc: e.matmul(PSF[bi][:], lhsT=yl[:, cc, s * 128:(s + 1) * 128],
                                                                       rhs=wo[:, cc, dh * 512:(dh + 1) * 512],
                                                                       start=(cc == 0), stop=(cc == 7)), r=[yl_T, wo_T], w=[PSF_T[bi]])
                                op("dve", lambda e: e.tensor_tensor(out=xt[:, dh * 512:(dh + 1) * 512], in0=xt[:, dh * 512:(dh + 1) * 512],
                                                                    in1=PSF[bi][:], op=ALU.add), r=[xt_T, PSF_T[bi]], w=[xt_T])
                            dma("sp", xres_d[i * 128:(i + 1) * 128, :], xt[:], r=[xt_T], w=[xres_T[i]])
                            if not moe:
                                hn, hn_T = hnr.next()
                                norm_tile(xt[:], xt_T, hn[:], hn_T, smr, junk, junk_T)
                                to_HT(hn, hn_T, i)
                            else:
                                h32, h32_T = hn32.next()
                                norm_tile(xt[:], xt_T, h32[:], h32_T, smr, junk, junk_T)
                                acopy(H3[:, i, :], h32[:], [h32_T], [H3_T[i]], eng="pool")
                                for k in range(8):
                                    pf = PSF[4 + k // 4]
                                    op("pe", lambda e, k=k: e.transpose(out=pf[:, (k % 4) * 128:(k % 4 + 1) * 128],
                                                                        in_=h32[:, k * 128:(k + 1) * 128], identity=ident),
                                       r=[h32_T, cst_T], w=[PSF_T[4 + k // 4]])
                                hT3, hT3_T = h3T.next()
                                acopy(hT3[:, 0:4, :], PSF[4][:].rearrange("p (k t) -> p k t", k=4), [PSF_T[4]], [hT3_T])
                                acopy(hT3[:, 4:8, :], PSF[5][:].rearrange("p (k t) -> p k t", k=4), [PSF_T[5]], [hT3_T], eng="dve")
                                pl, pl_T = PSB[0], PSB_T[0]
                                plf = PSF[(2 * i + 2) % 4]
                                plf_T = PSF_T[(2 * i + 2) % 4]
                                for kc in range(8):
                                    op("pe", lambda e, kc=kc: e.matmul(plf[:, 0:NE], lhsT=hT3[:, kc, :], rhs=wr[:, kc, :],
                                                                       start=(kc == 0), stop=(kc == 7)), r=[hT3_T, wr_T], w=[plf_T])
                                lg, lg_T = lgr.next()
                                acopy(lg[:, 0:8], plf[:, 0:NE], [plf_T], [lg_T])
                                dv = lambda fn, r=(), w=(): op("dve", fn, r=list(r), w=list(w))
                                dv(lambda e: e.max(out=lg[:, 8:16], in_=lg[:, 0:8]), [lg_T], [lg_T])
                                dv(lambda e: e.tensor_scalar(out=M1m[:, i, :], in0=lg[:, 0:8], scalar1=lg[:, 8:9], scalar2=None,
                                                             op0=ALU.is_ge), [lg_T], [rt_T])
                                dv(lambda e: e.tensor_scalar(out=lg[:, 16:24], in0=lg[:, 0:8], scalar1=lg[:, 9:10], scalar2=None,
                                                             op0=ALU.is_ge), [lg_T], [lg_T])
                                dv(lambda e: e.tensor_tensor(out=M2m[:, i, :], in0=lg[:, 16:24], in1=M1m[:, i, :], op=ALU.subtract),
                                   [lg_T, rt_T], [rt_T])
                                dv(lambda e: e.tensor_tensor(out=lg[:, 10:11], in0=lg[:, 9:10], in1=lg[:, 8:9], op=ALU.subtract),
                                   [lg_T], [lg_T])
                                op("act", lambda e: e.activation(out=lg[:, 10:11], in_=lg[:, 10:11], func=AF.Exp), r=[lg_T], w=[lg_T])
                                dv(lambda e: e.tensor_scalar(out=lg[:, 11:12], in0=lg[:, 10:11], scalar1=1.0, scalar2=None, op0=ALU.add),
                                   [lg_T], [lg_T])
                                dv(lambda e: e.reciprocal(out=GT[:, i, 0:1], in_=lg[:, 11:12]), [lg_T], [rt_T])
                                dv(lambda e: e.tensor_tensor(out=GT[:, i, 1:2], in0=lg[:, 10:11], in1=GT[:, i, 0:1], op=ALU.mult),
                                   [lg_T, rt_T], [rt_T])
                                prk = PSF[(2 * i + 3) % 4]
                                prk_T = PSF_T[(2 * i + 3) % 4]
                                op("pe", lambda e: e.matmul(prk[:, 0:8], lhsT=SUm, rhs=lg[:, 16:24], start=True, stop=True),
                                   r=[cst_T, lg_T], w=[prk_T])
                                op("pe", lambda e: e.matmul(prk[:, 8:16], lhsT=ones, rhs=lg[:, 16:24], start=True, stop=True),
                                   r=[cst_T, lg_T], w=[prk_T])
                                dv(lambda e: e.tensor_tensor(out=RK[:, i, :], in0=prk[:, 0:8], in1=tot[:], op=ALU.add),
                                   [prk_T, tot_T], [rt_T])
                                dv(lambda e: e.tensor_tensor(out=tot[:], in0=tot[:], in1=prk[:, 8:16], op=ALU.add),
                                   [prk_T, tot_T], [tot_T])
                    if moe:
                        zt = sb("m_zero", [128, 8, D], BF16, ps); zt_T = T()
                        op("pool", lambda e: e.memset(zt[:], 0.0), w=[zt_T])
                        hmz_T = T("hmz")
                        ybz_T = T("ybz")
                        hm_v = hm_d.rearrange("(b p) d -> p b d", p=128)
                        for b0 in range(0, NBLK + 1, 8):
                            nb = min(8, NBLK + 1 - b0)
                            dma("sp", hm_v[:, b0:b0 + nb, :], zt[:, 0:nb, :], r=[zt_T], w=[hmz_T] if b0 == 0 else [T()])
                        hmz_all = hmz_T
                        zf = sb("m_zf", [128, D], F32, ps); zf_T = T()
                        op("pool", lambda e: e.memset(zf[:], 0.0), w=[zf_T])
                        dma("sp", yb_d[NROW:NROW + 128, :], zf[:], r=[zf_T], w=[ybz_T])
                        RS = sb("m_RS", [128, NT, NE], F32, ps)
                        prod = sb("m_prod", [128, NT, NE], F32, ps)
                        dstf = sb("m_dstf", [128, 2, NT], F32, ps)
                        rkf = sb("m_rkf", [128, NT], F32, ps)
                        stv = sb("m_stv", [128, NE], F32, ps)
                        for e_ in range(NE):
                            dv(lambda e, e_=e_: e.memset(stv[:, e_:e_ + 1], float(e_ * CAPROWS)), [], [rt_T])
                        dv(lambda e: e.tensor_tensor(out=RS[:], in0=RK[:], in1=bc(stv[:].rearrange("p (o e) -> p o e", o=1), [128, NT, NE]),
                                                     op=ALU.add), [rt_T], [rt_T])
                        for k, Mk in enumerate((M1m, M2m)):
                            dv(lambda e, Mk=Mk: e.tensor_tensor(out=prod[:], in0=RS[:], in1=Mk[:], op=ALU.mult), [rt_T], [rt_T])
                            dv(lambda e, k=k: e.tensor_reduce(out=dstf[:, k, :], in_=prod[:], axis=AX.X, op=ALU.add), [rt_T], [rt_T])
                            dv(lambda e, Mk=Mk: e.tensor_tensor(out=prod[:], in0=RK[:], in1=Mk[:], op=ALU.mult), [rt_T], [rt_T])
                            dv(lambda e: e.tensor_reduce(out=rkf[:], in_=prod[:], axis=AX.X, op=ALU.add), [rt_T], [rt_T])
                            dv(lambda e: e.tensor_scalar(out=rkf[:], in0=rkf[:], scalar1=float(CAPROWS), scalar2=1.0e6,
                                                         op0=ALU.is_ge, op1=ALU.mult), [rt_T], [rt_T])
                            dv(lambda e, k=k: e.tensor_tensor(out=dstf[:, k, :], in0=dstf[:, k, :], in1=rkf[:], op=ALU.add), [rt_T], [rt_T])
                            dv(lambda e, k=k: e.tensor_scalar(out=dstf[:, k, :], in0=dstf[:, k, :], scalar1=float(NROW), scalar2=None,
                                                              op0=ALU.min), [rt_T], [rt_T])
                        dv(lambda e: e.tensor_copy(out=desti[:], in_=dstf[:]), [rt_T], [rt_T])
                        hms_T = TL(2 * NT, "hms")
                        barrier()
                        for i in range(NT):
                            for k in range(2):
                                dma("pool", hm_d[:, :], H3[:, i, :], r=[H3_T[i], rt_T], w=[hms_T[2 * i + k]],
                                    indirect=dict(out_offset=bass.IndirectOffsetOnAxis(ap=desti[:, k, i:i + 1], axis=0), in_offset=None))
                    barrier()
                stop_at(2 + 10 * layer)

                if not moe:
                    with contextlib.ExitStack() as ps:
                        wd = sb("d_wd", [128, NFF, D], BF16, ps); wd_T = T()
                        dma("pool", wd[:], ffd_d.rearrange("(j p) d -> p j d", p=128), w=[wd_T])
                        aT = sb("d_aT", [128, NFF, 512], BF16, ps); aT_T = TL(NFF, "aT")
                        wgu = Ring(nc, ps, "d_wgu", [128, 2, 8, 256], BF16, 3)
                        sgr = Ring(nc, ps, "d_sg", [128, 512], F32, 2)
                        xin = Ring(nc, ps, "d_x", [128, D], F32, 3)
                        hnr = Ring(nc, ps, "d_hn", [128, D], BF16, 2)
                        smr = Ring(nc, ps, "d_sm", [128, 4], F32, 3)
                        junk = sb("d_junk", [128, D], BF16, ps); junk_T = T()
                        dma("sp", nwb[:], nw_d[:, 2, :], w=[nwb_T])
                        ffg_v = ffg_d.rearrange("(kc p) f -> p kc f", p=128)
                        ffu_v = ffu_d.rearrange("(kc p) f -> p kc f", p=128)
                        for tb in range(8):
                            tsl = slice(tb * 512, (tb + 1) * 512)
                            for j in range(NFF):
                                if j % 2 == 0:
                                    wt2, wt_T = wgu.next()
                                    dma("pool", wt2[:, 0], ffg_v[:, :, j * 128:(j + 2) * 128], w=[wt_T])
                                    dma("pool", wt2[:, 1], ffu_v[:, :, j * 128:(j + 2) * 128], w=[wt_T])
                                wt = wt2[:, :, :, (j % 2) * 128:(j % 2 + 1) * 128]
                                pg, pg_T = PSF[j % 2], PSF_T[j % 2]
                                pu, pu_T = PSF[2 + j % 2], PSF_T[2 + j % 2]
                                rT = [wt_T] + HT_T[tb * 4:(tb + 1) * 4]
                                for kc in range(8):
                                    op("pe", lambda e, kc=kc: e.matmul(pg[:], lhsT=wt[:, 0, kc, :], rhs=HT[:, kc, tsl],
                                                                       start=(kc == 0), stop=(kc == 7)), r=rT, w=[pg_T])
                                for kc in range(8):
                                    op("pe", lambda e, kc=kc: e.matmul(pu[:], lhsT=wt[:, 1, kc, :], rhs=HT[:, kc, tsl],
                                                                       start=(kc == 0), stop=(kc == 7)), r=rT, w=[pu_T])
                                sg, sg_T = sgr.next()
                                op("act", lambda e: e.activation(out=sg[:], in_=pg[:], func=AF.Silu), r=[pg_T], w=[sg_T])
                                op("dve", lambda e, j=j: e.tensor_tensor(out=aT[:, j, :], in0=sg[:], in1=pu[:], op=ALU.mult),
                                   r=[sg_T, pu_T], w=[aT_T[j]])
                            for s in range(4):
                                i = tb * 4 + s
                                xt, xt_T = xin.next()
                                dma("sp", xt[:], xres_d[i * 128:(i + 1) * 128, :], r=[xres_T[i]], w=[xt_T])
                                for dh in range(2):
                                    pd, pd_T = PSF[4 + dh], PSF_T[4 + dh]
                                    for j in range(NFF):
                                        op("pe", lambda e, j=j: e.matmul(pd[:], lhsT=aT[:, j, s * 128:(s + 1) * 128],
                                                                         rhs=wd[:, j, dh * 512:(dh + 1) * 512],
                                                                         start=(j == 0), stop=(j == NFF - 1)), r=[aT_T[j], wd_T], w=[pd_T])
                                    op("dve", lambda e: e.tensor_tensor(out=xt[:, dh * 512:(dh + 1) * 512], in0=xt[:, dh * 512:(dh + 1) * 512],
                                                                        in1=pd[:], op=ALU.add), r=[xt_T, pd_T], w=[xt_T])
                                dma("sp", xres_d[i * 128:(i + 1) * 128, :], xt[:], r=[xt_T], w=[xres_T[i]])
                                hn, hn_T = hnr.next()
                                norm_tile(xt[:], xt_T, hn[:], hn_T, smr, junk, junk_T)
                                to_HT(hn, hn_T, i)
                        barrier()
                else:
                    with contextlib.ExitStack() as ps:
                        XT = sb("e_XT", [128, CAP, 8, 128], BF16, ps); XT_T = TL(CAP, "XT")
                        aT = sb("e_aT", [128, NFE, CAP * 128], BF16, ps); aT_T = TL(NFE, "eaT")
                        xb = Ring(nc, ps, "e_xb", [128, D], BF16, 3)
                        wgu = Ring(nc, ps, "e_wgu", [128, 2, 8, 256], BF16, 4)
                        wdq = Ring(nc, ps, "e_wd", [128, NFE, 256], BF16, 2)
                        sgr = Ring(nc, ps, "e_sg", [128, 512], F32, 2)
                        yor = Ring(nc, ps, "e_yo", [128, 512], F32, 3)
                        yb_T = TL(NBLK, "yb")
                        for ex_ in range(NE):
                            for bb in range(CAP):
                                b = ex_ * CAP + bb
                                xt, xt_T = xb.next()
                                dma("sp", xt[:], hm_d[b * 128:(b + 1) * 128, :], r=[hmz_all] + hms_T, w=[xt_T])
                                pb, pb_T = PSB[bb % 2], PSB_T[bb % 2]
                                for k in range(8):
                                    op("pe", lambda e, k=k: e.transpose(out=pb[:, k * 128:(k + 1) * 128], in_=xt[:, k * 128:(k + 1) * 128],
                                                                        identity=identb[:]), r=[xt_T, identb_T], w=[pb_T])
                                acopy(XT[:, bb, :, :], pb[:].rearrange("p (k t) -> p k t", k=8), [pb_T], [XT_T[bb]],
                                      eng=("act" if bb % 2 == 0 else "dve"))
                            mg_v = mg_d[ex_].rearrange("(kc p) f -> p kc f", p=128)
                            mu_v = mu_d[ex_].rearrange("(kc p) f -> p kc f", p=128)
                            md_v = md_d[ex_].rearrange("(j p) d -> p j d", p=128)
                            for j in range(NFE):
                                if j % 2 == 0:
                                    wt2, wt_T = wgu.next()
                                    dma("pool", wt2[:, 0], mg_v[:, :, j * 128:(j + 2) * 128], w=[wt_T])
                                    dma("pool", wt2[:, 1], mu_v[:, :, j * 128:(j + 2) * 128], w=[wt_T])
                                wt = wt2[:, :, :, (j % 2) * 128:(j % 2 + 1) * 128]
                                for qd in range(CAP // 4):
                                    pg, pg_T = PSF[qd % 2], PSF_T[qd % 2]
                                    pu, pu_T = PSF[2 + qd % 2], PSF_T[2 + qd % 2]
                                    rT = [wt_T] + XT_T[qd * 4:(qd + 1) * 4]
                                    for gu, pp in ((0, pg), (1, pu)):
                                        for kc in range(8):
                                            op("pe", lambda e, kc=kc, gu=gu, pp=pp: e.matmul(
                                                pp[:].rearrange("p (b t) -> p b t", b=4), lhsT=wt[:, gu, kc, :],
                                                rhs=XT[:, qd * 4:(qd + 1) * 4, kc, :], start=(kc == 0), stop=(kc == 7)),
                                               r=rT, w=[pg_T if gu == 0 else pu_T])
                                    sg, sg_T = sgr.next()
                                    op("act", lambda e: e.activation(out=sg[:], in_=pg[:], func=AF.Silu), r=[pg_T], w=[sg_T])
                                    op("dve", lambda e, j=j, qd=qd: e.tensor_tensor(out=aT[:, j, qd * 512:(qd + 1) * 512], in0=sg[:],
                                                                                     in1=pu[:], op=ALU.mult),
                                       r=[sg_T, pu_T], w=[aT_T[j]])
                            for dq in range(4):
                                wq, wq_T = wdq.next()
                                dma("pool", wq[:], md_v[:, :, dq * 256:(dq + 1) * 256], w=[wq_T])
                                for b2 in range(CAP // 2):
                                    pd, pd_T = PSF[4 + b2 % 2], PSF_T[4 + b2 % 2]
                                    for u in range(2):
                                        bb = b2 * 2 + u
                                        for j in range(NFE):
                                            op("pe", lambda e, j=j, u=u, bb=bb: e.matmul(
                                                pd[:, u * 256:(u + 1) * 256], lhsT=aT[:, j, bb * 128:(bb + 1) * 128], rhs=wq[:, j, :],
                                                start=(j == 0), stop=(j == NFE - 1)), r=[aT_T[j], wq_T], w=[pd_T])
                                    yo, yo_T = yor.next()
                                    acopy(yo[:], pd[:], [pd_T], [yo_T], eng=("act" if b2 % 2 == 0 else "dve"))
                                    for u in range(2):
                                        b = ex_ * CAP + b2 * 2 + u
                                        dma("sp", yb_d[b * 128:(b + 1) * 128, dq * 256:(dq + 1) * 256], yo[:, u * 256:(u + 1) * 256],
                                            r=[yo_T], w=[yb_T[b]] if dq == 3 else [T()])
                        barrier()
                    with contextlib.ExitStack() as ps:
                        xin = Ring(nc, ps, "f_x", [128, D], F32, 3)
                        y0r = Ring(nc, ps, "f_y0", [128, D], F32, 2)
                        y1r = Ring(nc, ps, "f_y1", [128, D], F32, 2)
                        outr = Ring(nc, ps, "f_o", [128, D], F32, 2)
                        smr = Ring(nc, ps, "f_sm", [128, 4], F32, 3)
                        junk = sb("f_junk", [128, D], BF16, ps); junk_T = T()
                        dma("sp", nwb[:], nw_d[:, 4, :], w=[nwb_T])
                        for i in range(NT):
                            xt, xt_T = xin.next()
                            dma("sp", xt[:], xres_d[i * 128:(i + 1) * 128, :], r=[xres_T[i]], w=[xt_T])
                            ys = []
                            for k, rr in enumerate((y0r, y1r)):
                                yk, yk_T = rr.next()
                                dma("pool", yk[:], yb_d[:, :], r=yb_T + [ybz_T, rt_T], w=[yk_T],
                                    indirect=dict(out_offset=None, in_offset=bass.IndirectOffsetOnAxis(ap=desti[:, k, i:i + 1], axis=0)))
                                ys.append((yk, yk_T))
                            for k in range(2):
                                yk, yk_T = ys[k]
                                op("dve", lambda e, k=k, yk=yk: e.scalar_tensor_tensor(out=xt[:], in0=yk[:], scalar=GT[:, i, k:k + 1], in1=xt[:],
                                                                                      op0=ALU.mult, op1=ALU.add),
                                   r=[yk_T, xt_T, rt_T], w=[xt_T])
                            ot, ot_T = outr.next()
                            norm_tile(xt[:], xt_T, ot[:], ot_T, smr, junk, junk_T)
                            dma("sp", out_d[i * 128:(i + 1) * 128, :], ot[:], r=[ot_T], w=[T()])
                        barrier()
        except _Stop:
            pass
        S.finish()
        hstack.close()
        print("ops", S.nop, "waits", S.nwait)
    return nc


def _consts():
    c = np.zeros((128, 768), np.float32)
    i = np.arange(128)
    c[:, 0:128] = np.eye(128)
    c[:, 128:256] = (i[:, None] <= i[None, :])
    c[:, 256:384] = (i[None, :] < i[:, None])
    c[:, 384:512] = (i[:, None] < i[None, :])
    c[:, 512:640] = 1.0
    c[:, 640:768] = 1.0 / 256.0
    return c


def _win_perm():
    A_B, A_C, A_X, S_Z, XBC, DT, GLU = 0, 256, 512, 768, 1280, 2304, 2312
    cols = []
    for j in range(2):
        for base in (A_B, A_C, A_X):
            cols += list(range(base + j * 128, base + (j + 1) * 128))
    for j in range(2):
        cols += list(range(GLU + j * 128, GLU + (j + 1) * 128))
        cols += list(range(GLU + 256 + j * 128, GLU + 256 + (j + 1) * 128))
    for g in range(2):
        cols += list(range(XBC + g * 256, XBC + (g + 1) * 256))
        cols += list(range(XBC + 512 + g * 128, XBC + 512 + (g + 1) * 128))
        cols += list(range(XBC + 768 + g * 128, XBC + 768 + (g + 1) * 128))
    cols += list(range(S_Z, S_Z + 512))
    cols += list(range(DT, DT + 8))
    assert len(cols) == IN_COLS and len(set(cols)) == IN_COLS
    return np.array(cols)


def _prep(inp):
    f = lambda a: np.ascontiguousarray(np.asarray(a, dtype=np.float32))
    bcast = lambda v: np.broadcast_to(v, (128,) + v.shape)
    perm = _win_perm()
    d = {}
    d["cst"] = _consts()
    nw = np.stack([inp["norm_mix"][0], inp["norm_ffn"][0], inp["norm_mix"][1], inp["norm_ffn"][1], inp["norm_final"]])
    d["nw"] = f(bcast(nw))
    for l in range(2):
        d[f"win{l}"] = f(inp["w_in"][l][:, perm])
    cwa = np.transpose(inp["conv_a_w"].reshape(2, 3, 2, 128), (3, 0, 2, 1))
    d["cwa"] = f(cwa)
    sw = np.concatenate([inp["conv_ssd_w"], inp["conv_ssd_b"][:, None, :]], axis=1)
    ch = []
    for g in range(2):
        ch += [g * 256 + np.arange(128), g * 256 + 128 + np.arange(128), 512 + g * 128 + np.arange(128), 768 + g * 128 + np.arange(128)]
    ch = np.stack(ch)
    d["cws"] = f(np.transpose(sw[:, :, ch], (3, 0, 2, 1)))
    cc = np.concatenate([inp["conv_conf_w"], inp["conv_conf_b"][:, None, :], inp["conf_ln_g"][:, None, :],
                         inp["conf_ln_b"][:, None, :]], axis=1)
    d["cwc"] = f(np.transpose(cc.reshape(2, 34, 2, 128), (3, 0, 2, 1)))
    hv = np.stack([inp["dt_bias"], inp["a_log"], inp["d_skip"]], axis=1)
    d["hv"] = f(bcast(hv))
    d["snw"] = f(bcast(inp["ssd_norm_w"]))
    d["wout"] = f(inp["w_out"])
    d["ffg"] = f(inp["ffn_w_gate"][0])
    d["ffu"] = f(inp["ffn_w_up"][0])
    d["ffd"] = f(inp["ffn_w_down"][0])
    d["wr"] = f(inp["moe_router"][0])
    d["mg"] = f(inp["moe_w_gate"][0])
    d["mu"] = f(inp["moe_w_up"][0])
    d["md"] = f(inp["moe_w_down"][0])
    return d


_NC = {}


def kernel(**inputs):
    inp = {k: np.asarray(v) for k, v in inputs.items()}
    if "nc" not in _NC:
        _NC["nc"] = build()
    nc = _NC["nc"]
    shared = _prep(inp)
    x = np.ascontiguousarray(inp["x"], dtype=np.float32)
    in_maps = [dict(shared, x=x[b]) for b in range(8)]
    res = run_bass_kernel_spmd(nc, in_maps, core_ids=list(range(8)))
    return np.stack([np.asarray(r["out"]) for r in res.results]).astype(np.float32)
```
